# Optimizing a Trainium2 kernel written in Bass

```python
import math
import jax, jax.numpy as jnp
from jax import lax
import numpy as np

D_MODEL = 2048
BATCH = 8
SEQ = 2048
DEPTH = 2

DN_HEADS = 4
DN_DK = 128
DN_DV = 128
DIFF_HEADS = 8
DIFF_DH = 64
DIFF_DV = 2 * DIFF_DH
ML_HEADS = 4
ML_DH = 128
DN_W = DN_HEADS * DN_DV
DIFF_W = DIFF_HEADS * DIFF_DV
ML_W = ML_HEADS * ML_DH
D_MIX = DN_W + DIFF_W + ML_W
CONV_K = 4
DN_CHUNK = 64
ML_CHUNK = 64
Q_BLOCK = 128
SPLIT_SIZES = (3 * DN_W, 2 * ML_W, DN_W, DN_HEADS, DN_HEADS, 3 * DIFF_W, ML_W, ML_W, ML_HEADS, ML_HEADS)
N_IN = sum(SPLIT_SIZES)
D_FF = 7 * D_MODEL // 2
N_EXPERTS = 8
TOP_K = 2
N_DENSE = (DEPTH + 1) // 2
N_MOE = DEPTH // 2
PLE_DIM = 256
EPS = 1e-6

kernel_name = "hybrid_deltanet_diffattn_mlstm_moe_block"


def rms_norm(x, w):
    xf = x.astype(jnp.float32)
    y = xf * lax.rsqrt(jnp.mean(xf * xf, axis=-1, keepdims=True) + EPS)
    return (y * w.astype(jnp.float32)).astype(x.dtype)


def l2_normalize(x):
    xf = x.astype(jnp.float32)
    return xf * lax.rsqrt(jnp.sum(xf * xf, axis=-1, keepdims=True) + EPS)


def causal_dwconv_silu(x, w):
    y = lax.conv_general_dilated(
        x, w[:, None, :].astype(x.dtype), window_strides=(1,),
        padding=[(w.shape[0] - 1, 0)], dimension_numbers=("NWC", "WIO", "NWC"),
        feature_group_count=x.shape[-1])
    return jax.nn.silu(y)


def to_chunks(t, chunk):
    b, s, h = t.shape[:3]
    t = t.astype(jnp.float32).reshape((b, s // chunk, chunk, h) + t.shape[3:])
    return jnp.moveaxis(t, 3, 1)


def from_chunks(o):
    b, h, n, c, d = o.shape
    return jnp.moveaxis(o, 1, 3).reshape(b, n * c, h, d)


def gated_delta_rule(q, k, v, g, beta):
    B, S, H, dk = q.shape
    dv = v.shape[-1]
    C = DN_CHUNK
    q = to_chunks(l2_normalize(q), C) * (dk ** -0.5)
    k = to_chunks(l2_normalize(k), C)
    v = to_chunks(v, C)
    beta = to_chunks(beta, C)
    g = jnp.cumsum(to_chunks(g, C), axis=-1)
    tri_incl = jnp.tril(jnp.ones((C, C), bool))
    tri_strict = jnp.tril(jnp.ones((C, C), bool), -1)
    decay = jnp.exp(jnp.where(tri_incl, g[..., :, None] - g[..., None, :], -jnp.inf))
    k_beta = k * beta[..., None]
    a = jnp.where(tri_strict, jnp.einsum('bhnid,bhnjd->bhnij', k_beta, k) * decay, 0.0)
    rhs = jnp.concatenate([v * beta[..., None], k_beta * jnp.exp(g)[..., None]], axis=-1)
    sol = lax.linalg.triangular_solve(a, rhs, left_side=True, lower=True, unit_diagonal=True)
    u, w = sol[..., :dv], sol[..., dv:]
    attn = jnp.einsum('bhnid,bhnjd->bhnij', q, k) * decay
    q_g = q * jnp.exp(g)[..., None]
    k_g = k * jnp.exp(g[..., -1:] - g)[..., None]
    g_last = jnp.exp(g[..., -1])

    def step(state, xs):
        u_c, w_c, attn_c, qg_c, kg_c, gl_c = xs
        v_new = u_c - w_c @ state
        o = qg_c @ state + attn_c @ v_new
        state = state * gl_c[..., None, None] + jnp.swapaxes(kg_c, -1, -2) @ v_new
        return state, o

    xs = tuple(jnp.moveaxis(t, 2, 0) for t in (u, w, attn, q_g, k_g, g_last))
    _, o = lax.scan(step, jnp.zeros((B, H, dk, dv), jnp.float32), xs)
    return from_chunks(jnp.moveaxis(o, 0, 2))


def mlstm_chunked(q, k, v, i_pre, log_f):
    B, S, H, d = q.shape
    C = ML_CHUNK
    q = to_chunks(q, C)
    k = to_chunks(k, C) * (d ** -0.5)
    v = to_chunks(v, C)
    i_pre = to_chunks(i_pre, C)
    b = jnp.cumsum(to_chunks(log_f, C), axis=-1)
    tri = jnp.tril(jnp.ones((C, C), bool))
    dlog = jnp.where(tri, b[..., :, None] - b[..., None, :] + i_pre[..., None, :], -jnp.inf)
    dmax = jnp.max(dlog, axis=-1)
    qk = jnp.einsum('bhnid,bhnjd->bhnij', q, k)
    a = b[..., -1:] - b + i_pre
    a_max = jnp.max(a, axis=-1)
    b_last = b[..., -1]

    def step(carry, xs):
        c_st, n_st, m_st = carry
        q_c, k_c, v_c, qk_c, dlog_c, dmax_c, b_c, a_c, amax_c, bl_c = xs
        m_t = jnp.maximum(dmax_c, b_c + m_st[..., None])
        s = qk_c * jnp.exp(dlog_c - m_t[..., None])
        inter = jnp.exp(b_c + m_st[..., None] - m_t)
        num = s @ v_c + inter[..., None] * (q_c @ c_st)
        den = jnp.sum(s, axis=-1) + inter * jnp.einsum('bhcd,bhd->bhc', q_c, n_st)
        h = num / jnp.maximum(jnp.abs(den), jnp.exp(-m_t))[..., None]
        m_new = jnp.maximum(bl_c + m_st, amax_c)
        carry_scale = jnp.exp(bl_c + m_st - m_new)
        wk = k_c * jnp.exp(a_c - m_new[..., None])[..., None]
        c_new = carry_scale[..., None, None] * c_st + jnp.swapaxes(wk, -1, -2) @ v_c
        n_new = carry_scale[..., None] * n_st + jnp.sum(wk, axis=-2)
        return (c_new, n_new, m_new), h

    xs = tuple(jnp.moveaxis(t, 2, 0) for t in (q, k, v, qk, dlog, dmax, b, a, a_max, b_last))
    init = (jnp.zeros((B, H, d, d), jnp.float32), jnp.zeros((B, H, d), jnp.float32),
            jnp.zeros((B, H), jnp.float32))
    _, h = lax.scan(step, init, xs)
    return from_chunks(jnp.moveaxis(h, 0, 2))


def diff_attention(q, k, v, lam):
    B, S, H, _, dh = q.shape
    nb = S // Q_BLOCK
    slopes = 2.0 ** (-8.0 * jnp.arange(1, H + 1, dtype=jnp.float32) / H)
    qb = q.reshape(B, nb, Q_BLOCK, H, 2, dh).transpose(1, 0, 3, 4, 2, 5) * (dh ** -0.5)
    kt = k.transpose(0, 2, 3, 1, 4)
    vt = v.transpose(0, 2, 1, 3)
    pos_k = jnp.arange(S)

    def block(args):
        q_blk, blk = args
        pos_q = blk * Q_BLOCK + jnp.arange(Q_BLOCK)
        dist = pos_q[:, None] - pos_k[None, :]
        s = jnp.einsum('bhjqd,bhjkd->bhjqk', q_blk, kt).astype(jnp.float32)
        s = s - slopes[:, None, None, None] * dist.astype(jnp.float32)
        s = jnp.where(dist >= 0, s, -jnp.inf)
        pr = jax.nn.softmax(s, axis=-1)
        a = pr[:, :, 0] - lam * pr[:, :, 1]
        return jnp.einsum('bhqk,bhkd->bhqd', a.astype(vt.dtype), vt)

    o = lax.map(block, (qb, jnp.arange(nb)))
    return o.transpose(1, 0, 3, 2, 4).reshape(B, S, H, -1)


def hybrid_mixer(hn, w_in, conv_dn, conv_ml, dn_a_log, dn_dt_bias, dn_norm,
                 lq1, lk1, lq2, lk2, diff_norm, ml_i_bias, ml_f_bias, ml_norm, lambda_init):
    B, S, _ = hn.shape
    f32 = jnp.float32
    z = hn @ w_in
    split_points = [int(c) for c in np.cumsum(SPLIT_SIZES)[:-1]]
    (dn_qkv, ml_qk, dn_z, dn_b, dn_a, diff_qkv, ml_v, ml_o, ml_i, ml_f) = jnp.split(z, split_points, axis=-1)

    dn_qkv = causal_dwconv_silu(dn_qkv, conv_dn)
    dq, dkk, dvv = jnp.split(dn_qkv, 3, axis=-1)
    beta = jax.nn.sigmoid(dn_b.astype(f32))
    g = -jnp.exp(dn_a_log) * jax.nn.softplus(dn_a.astype(f32) + dn_dt_bias)
    o_dn = gated_delta_rule(dq.reshape(B, S, DN_HEADS, DN_DK), dkk.reshape(B, S, DN_HEADS, DN_DK),
                            dvv.reshape(B, S, DN_HEADS, DN_DV), g, beta)
    o_dn = rms_norm(o_dn, dn_norm) * jax.nn.silu(dn_z.reshape(B, S, DN_HEADS, DN_DV).astype(f32))

    aq, ak, av = jnp.split(diff_qkv, 3, axis=-1)
    lam = (jnp.exp(jnp.sum(lq1.astype(f32) * lk1.astype(f32)))
           - jnp.exp(jnp.sum(lq2.astype(f32) * lk2.astype(f32))) + lambda_init)
    o_diff = diff_attention(aq.reshape(B, S, DIFF_HEADS, 2, DIFF_DH), ak.reshape(B, S, DIFF_HEADS, 2, DIFF_DH),
                            av.reshape(B, S, DIFF_HEADS, DIFF_DV), lam)
    o_diff = rms_norm(o_diff, diff_norm).astype(f32) * (1.0 - lambda_init)

    ml_qk = causal_dwconv_silu(ml_qk, conv_ml)
    mq, mk = jnp.split(ml_qk, 2, axis=-1)
    i_pre = ml_i.astype(f32) + ml_i_bias
    log_f = jax.nn.log_sigmoid(ml_f.astype(f32) + ml_f_bias)
    o_ml = mlstm_chunked(mq.reshape(B, S, ML_HEADS, ML_DH), mk.reshape(B, S, ML_HEADS, ML_DH),
                         ml_v.reshape(B, S, ML_HEADS, ML_DH), i_pre, log_f)
    o_ml = jax.nn.sigmoid(ml_o.reshape(B, S, ML_HEADS, ML_DH).astype(f32)) * rms_norm(o_ml, ml_norm)

    return jnp.concatenate([o_dn.reshape(B, S, DN_W), o_diff.reshape(B, S, DIFF_W),
                            o_ml.reshape(B, S, ML_W)], axis=-1).astype(hn.dtype)


def swiglu(x, w_gate, w_up, w_down):
    return (jax.nn.silu(x @ w_gate) * (x @ w_up)) @ w_down


def moe_swiglu(x, router, w_gate, w_up, w_down):
    B, S, D = x.shape
    xt = x.reshape(B * S, D)
    logits = (xt @ router).astype(jnp.float32)
    top_v, top_i = lax.top_k(logits, TOP_K)
    top_w = jax.nn.softmax(top_v, axis=-1)
    gates = jnp.sum(jax.nn.one_hot(top_i, N_EXPERTS, dtype=jnp.float32) * top_w[..., None], axis=1)
    y = jnp.zeros_like(xt)
    for e in range(N_EXPERTS):
        y = y + gates[:, e:e + 1].astype(xt.dtype) * swiglu(xt, w_gate[e], w_up[e], w_down[e])
    return y.reshape(B, S, D)


def setup_inputs(seed: int = 0) -> dict:
    key = jax.random.key(seed)
    ks = iter(jax.random.split(key, 32))
    f32 = jnp.float32

    def nrm(shape, scale):
        return jax.random.normal(next(ks), shape, f32) * scale

    def gain(shape):
        return 1.0 + nrm(shape, 0.05)

    x = nrm((BATCH, SEQ, D_MODEL), 1.0)
    p = nrm((DEPTH, BATCH, SEQ, PLE_DIM), 1.0)
    attn_norm = gain((DEPTH, D_MODEL))
    w_in = nrm((DEPTH, D_MODEL, N_IN), D_MODEL ** -0.5)
    conv_dn = nrm((DEPTH, CONV_K, 3 * DN_W), CONV_K ** -0.5)
    conv_ml = nrm((DEPTH, CONV_K, 2 * ML_W), CONV_K ** -0.5)
    dn_a_log = jnp.log(jax.random.uniform(next(ks), (DEPTH, DN_HEADS), f32, 1.0, 16.0))
    dn_dt_bias = nrm((DEPTH, DN_HEADS), 0.1)
    dn_norm = gain((DEPTH, DN_DV))
    diff_lq1 = nrm((DEPTH, DIFF_DH), 0.1)
    diff_lk1 = nrm((DEPTH, DIFF_DH), 0.1)
    diff_lq2 = nrm((DEPTH, DIFF_DH), 0.1)
    diff_lk2 = nrm((DEPTH, DIFF_DH), 0.1)
    diff_norm = gain((DEPTH, DIFF_DV))
    ml_i_bias = nrm((DEPTH, ML_HEADS), 0.1)
    ml_f_bias = 3.0 + 3.0 * jax.random.uniform(next(ks), (DEPTH, ML_HEADS), f32)
    ml_norm = gain((DEPTH, ML_DH))
    w_out = nrm((DEPTH, D_MIX, D_MODEL), D_MIX ** -0.5)
    ffn_norm = gain((DEPTH, D_MODEL))
    dense_w_gate = nrm((N_DENSE, D_MODEL, D_FF), D_MODEL ** -0.5)
    dense_w_up = nrm((N_DENSE, D_MODEL, D_FF), D_MODEL ** -0.5)
    dense_w_down = nrm((N_DENSE, D_FF, D_MODEL), D_FF ** -0.5)
    router = nrm((N_MOE, D_MODEL, N_EXPERTS), D_MODEL ** -0.5)
    moe_w_gate = nrm((N_MOE, N_EXPERTS, D_MODEL, D_FF), D_MODEL ** -0.5)
    moe_w_up = nrm((N_MOE, N_EXPERTS, D_MODEL, D_FF), D_MODEL ** -0.5)
    moe_w_down = nrm((N_MOE, N_EXPERTS, D_FF, D_MODEL), D_FF ** -0.5)
    ple_norm = gain((DEPTH, D_MODEL))
    ple_proj = nrm((DEPTH, PLE_DIM, D_MODEL), PLE_DIM ** -0.5)
    ple_gate = nrm((DEPTH, D_MODEL, D_MODEL), D_MODEL ** -0.5)
    final_norm = gain((D_MODEL,))
    return {"x": x, "p": p, "attn_norm": attn_norm, "w_in": w_in, "conv_dn": conv_dn,
            "conv_ml": conv_ml, "dn_a_log": dn_a_log, "dn_dt_bias": dn_dt_bias, "dn_norm": dn_norm,
            "diff_lq1": diff_lq1, "diff_lk1": diff_lk1, "diff_lq2": diff_lq2, "diff_lk2": diff_lk2,
            "diff_norm": diff_norm, "ml_i_bias": ml_i_bias, "ml_f_bias": ml_f_bias, "ml_norm": ml_norm,
            "w_out": w_out, "ffn_norm": ffn_norm, "dense_w_gate": dense_w_gate, "dense_w_up": dense_w_up,
            "dense_w_down": dense_w_down, "router": router, "moe_w_gate": moe_w_gate,
            "moe_w_up": moe_w_up, "moe_w_down": moe_w_down, "ple_norm": ple_norm,
            "ple_proj": ple_proj, "ple_gate": ple_gate, "final_norm": final_norm}


def reference(x, p, attn_norm, w_in, conv_dn, conv_ml, dn_a_log, dn_dt_bias, dn_norm,
              diff_lq1, diff_lk1, diff_lq2, diff_lk2, diff_norm, ml_i_bias, ml_f_bias, ml_norm,
              w_out, ffn_norm, dense_w_gate, dense_w_up, dense_w_down, router, moe_w_gate,
              moe_w_up, moe_w_down, ple_norm, ple_proj, ple_gate, final_norm):
    h = x
    for i in range(DEPTH):
        lambda_init = 0.8 - 0.6 * math.exp(-0.3 * i)
        hn = rms_norm(h, attn_norm[i])
        mix = hybrid_mixer(hn, w_in[i], conv_dn[i], conv_ml[i], dn_a_log[i], dn_dt_bias[i], dn_norm[i],
                           diff_lq1[i], diff_lk1[i], diff_lq2[i], diff_lk2[i], diff_norm[i],
                           ml_i_bias[i], ml_f_bias[i], ml_norm[i], lambda_init)
        h = h + mix @ w_out[i]
        hn = rms_norm(h, ffn_norm[i])
        j = i // 2
        if i % 2 == 0:
            f = swiglu(hn, dense_w_gate[j], dense_w_up[j], dense_w_down[j])
        else:
            f = moe_swiglu(hn, router[j], moe_w_gate[j], moe_w_up[j], moe_w_down[j])
        h = h + f
        gate = jax.nn.sigmoid(rms_norm(h, ple_norm[i]) @ ple_gate[i])
        h = h + (p[i] @ ple_proj[i]) * gate
    return rms_norm(h, final_norm)
```

```python
import math
from contextlib import ExitStack
import numpy as np
import concourse.bass as bass
import concourse.mybir as mybir
from concourse.bass_utils import run_bass_kernel_spmd

F32 = mybir.dt.float32
BF16 = mybir.dt.bfloat16
AF = mybir.ActivationFunctionType
ALU = mybir.AluOpType
EPS = 1e-6
NH = 4
NDH = 8
SLOT = 8192
NSLOT = 3


class Cfg:
    def __init__(self, D=2048, T=2048, FF=7168, E=8, PLE=256, L=2, TQ=512):
        self.D, self.T, self.FF, self.E, self.PLE, self.L, self.TQ = D, T, FF, E, PLE, L, TQ
        self.DC = D // 128
        self.FC = FF // 128
        self.NQ = T // TQ
        self.NCH = T // 128
        self.QG = min(512, T)
        self.NIN = 7184


class Sched:
    def __init__(self, nc, es):
        self.nc = nc
        self.es = es
        self.eng = {"pe": nc.tensor, "dve": nc.vector, "act": nc.scalar, "pool": nc.gpsimd, "sp": nc.sync}
        self.sem = {}
        self.cnt = {}
        for e in self.eng:
            self.sem["e:" + e] = es.enter_context(nc.semaphore("sem_" + e))
            self.cnt["e:" + e] = 0
        self.seen = {}
        self.last_w = {}
        self.readers = {}
        self.nbank = 0
        self.nslot = 0
        self.ndsem = 0

    def dsem(self, name):
        k = "d:" + name
        if k not in self.sem:
            self.sem[k] = self.es.enter_context(self.nc.semaphore("dsem_%d" % self.ndsem))
            self.ndsem += 1
            self.cnt[k] = 0
        return k

    def _deps(self, reads, writes):
        deps = {}
        def add(tok):
            if tok is None:
                return
            k, v = tok
            if deps.get(k, 0) < v:
                deps[k] = v
        for k in reads:
            add(self.last_w.get(k))
        for k in writes:
            add(self.last_w.get(k))
            for t in self.readers.get(k, ()):
                add(t)
        return deps

    def _wait(self, e, deps):
        eng = self.eng[e]
        for k, v in deps.items():
            if k == "e:" + e and e == "pe":
                continue
            if self.seen.get((e, k), 0) >= v:
                continue
            eng.wait_ge(self.sem[k], v)
            self.seen[(e, k)] = v

    def _record(self, tok, reads, writes):
        for k in writes:
            self.last_w[k] = tok
            self.readers[k] = []
        for k in reads:
            if k in writes:
                continue
            self.readers.setdefault(k, []).append(tok)
            if len(self.readers[k]) > 64:
                best = {}
                for (kk, vv) in self.readers[k]:
                    if best.get(kk, 0) < vv:
                        best[kk] = vv
                self.readers[k] = list(best.items())

    def op(self, e, fn, reads=(), writes=()):
        deps = self._deps(reads, writes)
        self._wait(e, deps)
        ins = fn(self.eng[e])
        k = "e:" + e
        self.cnt[k] += 1
        ins.then_inc(self.sem[k], 1)
        self._record((k, self.cnt[k]), reads, writes)

    def dma(self, e, out, in_, reads=(), writes=(), sem=None, **kw):
        deps = self._deps(reads, writes)
        self._wait(e, deps)
        k = self.dsem(sem)
        ins = self.eng[e].dma_start(out=out, in_=in_, **kw)
        self.cnt[k] += 16
        ins.then_inc(self.sem[k], 16)
        self._record((k, self.cnt[k]), reads, writes)

    def bank(self):
        b = self.nbank % 8
        self.nbank += 1
        return b

    def slot(self):
        s = self.nslot % NSLOT
        self.nslot += 1
        return s

    def barrier(self):
        allv = {k: v for k, v in self.cnt.items() if v > 0}
        for e in self.eng:
            self._wait(e, dict(allv))
        self.last_w.clear()
        self.readers.clear()

    def finish(self, e="sp"):
        allv = {k: v for k, v in self.cnt.items() if v > 0}
        self._wait(e, allv)


class Prog:
    def __init__(self, cfg):
        self.cfg = cfg
        self.nc = bass.Bass("TRN2", target_bir_lowering=False)
        self.es = ExitStack()
        self.S = None

    def din(self, name, shape, dt=F32):
        return self.nc.dram_tensor(name, list(shape), dt, kind="ExternalInput").ap()

    def dscr(self, name, shape, dt=F32):
        return self.nc.dram_tensor(name, list(shape), dt).ap()

    def sb(self, st, name, shape, dt=F32):
        self._nid = getattr(self, "_nid", 0) + 1
        return st.enter_context(self.nc.sbuf_tensor("%s_%d" % (name, self._nid), list(shape), dt))

    def build(self):
        cfg = self.cfg
        nc = self.nc
        D, T, FF, E, PLE, L, TQ = cfg.D, cfg.T, cfg.FF, cfg.E, cfg.PLE, cfg.L, cfg.TQ
        DC, FC, NQ, NCH = cfg.DC, cfg.FC, cfg.NQ, cfg.NCH
        NDENSE, NMOE = (L + 1) // 2, L // 2
        I = {}
        I["x"] = self.din("x", [T, D])
        I["p"] = self.din("p", [L, T, PLE])
        I["attn_norm"] = self.din("attn_norm", [L, 128, DC])
        I["w_in"] = self.din("w_in", [L, D, cfg.NIN])
        I["conv_dn"] = self.din("conv_dn", [L, 1536, 4])
        I["conv_ml"] = self.din("conv_ml", [L, 1024, 4])
        for nm in ("dn_a_log", "dn_dt_bias", "ml_i_bias", "ml_f_bias"):
            I[nm] = self.din(nm, [L, 4, 1])
        for nm in ("dn_norm", "diff_norm", "ml_norm"):
            I[nm] = self.din(nm, [L, 128, 1])
        for nm in ("diff_lq1", "diff_lk1", "diff_lq2", "diff_lk2"):
            I[nm] = self.din(nm, [L, 64])
        I["w_out"] = self.din("w_out", [L, 2048, D])
        I["ffn_norm"] = self.din("ffn_norm", [L, 128, DC])
        I["dense_w_gate"] = self.din("dense_w_gate", [NDENSE, D, FF])
        I["dense_w_up"] = self.din("dense_w_up", [NDENSE, D, FF])
        I["dense_w_down"] = self.din("dense_w_down", [NDENSE, FF, D])
        I["router"] = self.din("router", [max(NMOE, 1), D, E])
        I["moe_w_gate"] = self.din("moe_w_gate", [max(NMOE, 1), E, D, FF])
        I["moe_w_up"] = self.din("moe_w_up", [max(NMOE, 1), E, D, FF])
        I["moe_w_down"] = self.din("moe_w_down", [max(NMOE, 1), E, FF, D])
        I["ple_norm"] = self.din("ple_norm", [L, 128, DC])
        I["ple_proj"] = self.din("ple_proj", [L, PLE, D])
        I["ple_gate"] = self.din("ple_gate", [L, D, D])
        I["final_norm"] = self.din("final_norm", [128, DC])
        I["c_ident"] = self.din("c_ident", [128, 128])
        I["c_mui"] = self.din("c_mui", [128, 128])
        I["c_mus"] = self.din("c_mus", [128, 128])
        I["c_cmask"] = self.din("c_cmask", [4, T])
        I["c_qaug"] = self.din("c_qaug", [NDH, 3, T])
        I["c_kaug"] = self.din("c_kaug", [NDH, 3, T])
        self.I = I
        self.out = nc.dram_tensor("out", [T, D], F32, kind="ExternalOutput").ap()
        self.hT = self.dscr("hT", [D, T])
        self.zT = self.dscr("zT", [7184, T])
        self.mixT = self.dscr("mixT", [2048, T], BF16)
        self.hnTd = self.dscr("hnTd", [D, T], BF16)
        self.gTd = self.dscr("gTd", [8, T])
        self.actd = self.dscr("actd", [E, FF, T], BF16)

        es = self.es
        S = self.S = Sched(nc, es)
        self.psum = es.enter_context(nc.psum_tensor("ps", [128, 8, 512], F32))
        self.ident = self.sb(es, "ident", [128, 128])
        self.ones32 = self.sb(es, "ones32", [128, 128])
        self.ones16 = self.sb(es, "ones16", [128, 128], BF16)
        self.mui = self.sb(es, "mui", [128, 128])
        self.mus = self.sb(es, "mus", [128, 128])
        self.mui16 = self.sb(es, "mui16", [128, 128], BF16)
        S.dma("sp", self.ident[:], I["c_ident"], writes=["ident"], sem="c_ident")
        S.dma("sp", self.mui[:], I["c_mui"], writes=["mui"], sem="c_mui")
        S.dma("sp", self.mus[:], I["c_mus"], writes=["mus"], sem="c_mus")
        S.op("dve", lambda e: e.memset(self.ones32[:], 1.0), writes=["ones32"])
        S.op("dve", lambda e: e.memset(self.ones16[:], 1.0), writes=["ones16"])
        S.op("dve", lambda e: e.tensor_copy(out=self.mui16[:], in_=self.mui[:]), reads=["mui"], writes=["mui16"])

        stop = ""
        self.token_phase(first=True, layer=0)
        for l in range(L):
            if stop == "tp0":
                break
            S.barrier()
            self.mixer_phase(l)
            if stop == "mix0":
                break
            S.barrier()
            self.token_phase(first=False, layer=l, mode="A")
            self.ffn_up_phase(l)
            self.token_phase(first=False, layer=l, mode="B")
            if stop == "tp1":
                break
        S.finish("sp")
        for e in ("pe", "dve", "act", "pool"):
            S.finish(e)
        self.es.close()
        return nc

    def ps(self, b, n=128, w=512):
        return self.psum[:n, b, :w]

    def evac_engine(self):
        self._ev = getattr(self, "_ev", 0) + 1
        return "act" if self._ev % 2 else "dve"

    def transpose_to(self, src_ap, src_key, np_in, nf_in, bank, col0=0):
        S = self.S
        out = self.psum[:nf_in, bank, col0:col0 + np_in]
        S.op("pe", lambda e: e.transpose(out, src_ap, self.ident[:np_in, :np_in]),
             reads=[src_key, "ident"], writes=["ps%d" % bank])

    def gemm_ksplit(self, K, w, ncols, rhs_fn, epi_fn):
        S, cfg = self.S, self.cfg
        KC = K // 128
        KB = SLOT // 512
        kblocks = [(k0, min(KB, KC - k0)) for k0 in range(0, KC, KB)]
        blocks = []
        for cg0 in range(0, ncols, 512):
            ncg = min(512, ncols - cg0)
            for kbi, (k0, nk) in enumerate(kblocks):
                blocks.append({"cg0": cg0, "ncg": ncg, "kbi": kbi, "k0": k0, "nk": nk})
        issued = [0]

        def issue(bi):
            blk = blocks[bi]
            s_ = S.slot()
            blk["slot"] = s_
            nk, ncg = blk["nk"], blk["ncg"]
            view = self.wslots[:, s_, 0:nk * ncg].rearrange("p (c n) -> p c n", n=ncg)
            blk["view"] = view
            src = w[blk["k0"] * 128:(blk["k0"] + nk) * 128, blk["cg0"]:blk["cg0"] + ncg].rearrange("(c p) n -> p c n", p=128)
            half = nk // 2 if nk >= 8 else nk
            first = True
            for a in range(0, nk, half):
                S.dma("pool", view[:, a:a + half, :], src[:, a:a + half, :], writes=["w%d" % s_] if first else [], sem="w%d" % s_)
                first = False
            k = S.dsem("w%d" % s_)
            S.last_w["w%d" % s_] = (k, S.cnt[k])

        banks = None
        for bi, blk in enumerate(blocks):
            while issued[0] < min(len(blocks), bi + NSLOT - 1):
                issue(issued[0])
                issued[0] += 1
            nch = blk["ncg"] // 128
            if blk["kbi"] == 0:
                banks = [S.bank() for _ in range(nch)]
            lastkb = blk["kbi"] == len(kblocks) - 1
            for ci in range(nch):
                b = banks[ci]
                rk = list(set(rhs_fn(blk["k0"] + kk)[1] for kk in range(blk["nk"])))

                def mm(e, blk=blk, ci=ci, b=b, lastkb=lastkb):
                    ins = None
                    for kk in range(blk["nk"]):
                        ins = e.matmul(self.psum[:, b, :cfg.TQ], lhsT=blk["view"][:, kk, ci * 128:(ci + 1) * 128],
                                       rhs=rhs_fn(blk["k0"] + kk)[0], start=(blk["kbi"] == 0 and kk == 0),
                                       stop=(lastkb and kk == blk["nk"] - 1))
                    return ins
                S.op("pe", mm, reads=["w%d" % blk["slot"]] + rk, writes=["ps%d" % b])
            if lastkb:
                for ci in range(nch):
                    epi_fn(blk["cg0"] // 128 + ci, banks[ci], 128)

    def gemm(self, K, chunks, rhs_fn, epi_fn, nparts=1):
        S, cfg = self.S, self.cfg
        KC = K // 128
        maxcols = SLOT // KC
        blocks = []
        for idx, (w, c0, n) in enumerate(chunks):
            if blocks and blocks[-1]["w"] is w and blocks[-1]["c0"] + blocks[-1]["n"] == c0 \
                    and blocks[-1]["n"] + n <= maxcols:
                blocks[-1]["ch"].append((idx, blocks[-1]["n"], n))
                blocks[-1]["n"] += n
            else:
                blocks.append({"w": w, "c0": c0, "n": n, "ch": [(idx, 0, n)]})
        issued = [0]

        def issue(bi):
            blk = blocks[bi]
            s = S.slot()
            blk["slot"] = s
            view = self.wslots[:, s, 0:KC * blk["n"]].rearrange("p (c n) -> p c n", n=blk["n"])
            blk["view"] = view
            src = blk["w"][:, blk["c0"]:blk["c0"] + blk["n"]].rearrange("(c p) n -> p c n", p=128)
            half = KC // 2 if KC >= 8 else KC
            for a in range(0, KC, half):
                S.dma("pool", view[:, a:a + half, :], src[:, a:a + half, :], writes=["w%d" % s] if a == 0 else [],
                      reads=[] if a == 0 else [], sem="w%d" % s)
            k = S.dsem("w%d" % s)
            S.last_w["w%d" % s] = (k, S.cnt[k])

        for bi, blk in enumerate(blocks):
            while issued[0] < min(len(blocks), bi + NSLOT - 1):
                issue(issued[0])
                issued[0] += 1
            for (idx, off, n) in blk["ch"]:
              for part in range(nparts):
                b = S.bank()
                rf = (lambda kc: rhs_fn(kc)) if nparts == 1 else (lambda kc, part=part: rhs_fn(kc, part))
                rk = [rf(kc)[1] for kc in range(KC)]

                def mm(e, blk=blk, off=off, n=n, b=b, rf=rf):
                    ins = None
                    for kc in range(KC):
                        ins = e.matmul(self.psum[:n, b, :cfg.TQ], lhsT=blk["view"][:, kc, off:off + n],
                                       rhs=rf(kc)[0], start=(kc == 0), stop=(kc == KC - 1))
                    return ins
                S.op("pe", mm, reads=["w%d" % blk["slot"]] + list(set(rk)), writes=["ps%d" % b])
                if nparts == 1:
                    epi_fn(idx, b, n)
                else:
                    epi_fn(idx, b, n, part)

    def norm(self, wname, layer, out_fn=None):
        S, cfg = self.S, self.cfg
        DC, TQ, D = cfg.DC, cfg.TQ, cfg.D
        wv = self.I[wname] if layer is None else self.I[wname][layer]
        S.dma("sp", self.nw[:], wv, writes=["nw"], sem="nw")
        b = S.bank()
        for kc in range(DC):
            sl = kc % 2
            S.op("act", lambda e, kc=kc, sl=sl: e.activation(out=self.sq[:, sl, :], in_=self.h_q[:, kc, :], func=AF.Square),
                 reads=["h%d" % kc], writes=["sq%d" % sl])
            S.op("pe", lambda e, kc=kc, sl=sl: e.matmul(self.psum[:, b, :TQ], lhsT=self.ones16[:], rhs=self.sq[:, sl, :],
                                                        start=(kc == 0), stop=(kc == DC - 1)),
                 reads=["sq%d" % sl, "ones16"], writes=["ps%d" % b] if kc == 0 else [])
        S.last_w["ps%d" % b] = ("e:pe", S.cnt["e:pe"])
        S.op("act", lambda e: e.activation(out=self.rstd[:], in_=self.psum[:, b, :TQ], func=AF.Sqrt, scale=1.0 / D, bias=self.epsb[:]),
             reads=["ps%d" % b, "epsb"], writes=["rstd"])
        S.op("dve", lambda e: e.reciprocal(out=self.rstd[:], in_=self.rstd[:]), reads=["rstd"], writes=["rstd"])
        for kc in range(DC):
            if out_fn is None:
                S.op("dve", lambda e, kc=kc: e.scalar_tensor_tensor(out=self.hn_q[:, kc, :], in0=self.h_q[:, kc, :],
                                                                    scalar=self.nw[:, kc:kc + 1], in1=self.rstd[:],
                                                                    op0=ALU.mult, op1=ALU.mult),
                     reads=["h%d" % kc, "nw", "rstd"], writes=["hn%d" % kc])
            else:
                out_fn(kc)

    def token_phase(self, first, layer, mode=""):
        S, cfg, I, nc = self.S, self.cfg, self.I, self.nc
        D, T, FF, E, PLE, L, TQ = cfg.D, cfg.T, cfg.FF, cfg.E, cfg.PLE, cfg.L, cfg.TQ
        DC, FC, NQ = cfg.DC, cfg.FC, cfg.NQ
        with ExitStack() as st:
            self.wslots = self.sb(st, "wslots", [128, NSLOT, SLOT], BF16)
            self.h_q = self.sb(st, "h_q", [128, DC, TQ])
            self.hn_q = self.sb(st, "hn_q", [128, DC, TQ], BF16)
            nbig = max(FC, 2 * DC, 16)
            self.big = self.sb(st, "big", [128, nbig, TQ], BF16)
            self.sq = self.sb(st, "sq", [128, 2, TQ], BF16)
            self.rstd = self.sb(st, "rstd", [128, TQ])
            self.nw = self.sb(st, "nw", [128, DC])
            self.epsb = self.sb(st, "epsb", [128, 1])
            self.stage = self.sb(st, "stage", [128, 4, TQ])
            self.tmp = self.sb(st, "tmp", [128, 2, TQ])
            self.xin = self.sb(st, "xin", [128, 2, 128])
            self.gbc = self.sb(st, "gbc", [128, 2, TQ])
            self.rt32 = self.sb(st, "rt32", [128, DC, E])
            self.lgT = self.sb(st, "lgT", [8, TQ])
            self.gT = self.sb(st, "gT", [8, TQ])
            self.lgw = self.sb(st, "lgw", [128, 8, 32])
            self.sel = self.sb(st, "sel", [8, E, 128])
            self.pT = self.sb(st, "pT", [128, max(PLE // 128, 1), TQ], BF16)
            pp32 = self.big[:].rearrange("p c t -> p (c t)").bitcast(F32)[:, 0:DC * TQ].rearrange("p (c t) -> p c t", t=TQ)
            S.op("dve", lambda e: e.memset(self.epsb[:], EPS), writes=["epsb"])
            for ee in range(E):
                S.op("dve", lambda e, ee=ee: e.tensor_scalar(out=self.sel[:, ee, :], in0=self.ones32[:8, :],
                                                            scalar1=self.ident[:8, ee:ee + 1], scalar2=None, op0=ALU.mult),
                     reads=["ones32", "ident"], writes=["sel"])
            self._stg = 0

            def hkeys():
                return ["h%d" % k for k in range(DC)]

            def hnrhs(kc):
                return (self.hn_q[:, kc, :], "hn%d" % kc)

            for q in range(NQ):
                t0 = q * TQ
                if first:
                    for tt in range(TQ // 128):
                        for dc in range(DC):
                            sl = (tt * DC + dc) % 2
                            S.dma("sp", self.xin[:, sl, :], I["x"][t0 + tt * 128:t0 + (tt + 1) * 128, dc * 128:(dc + 1) * 128],
                                  writes=["xin%d" % sl], sem="xin%d" % sl)
                            b = S.bank()
                            self.transpose_to(self.xin[:, sl, :], "xin%d" % sl, 128, 128, b)
                            S.op(self.evac_engine(), lambda e, dc=dc, tt=tt, b=b: (e.tensor_copy(out=self.h_q[:, dc, tt * 128:(tt + 1) * 128], in_=self.psum[:, b, :128])
                                                                                     if e is nc.vector else e.copy(out=self.h_q[:, dc, tt * 128:(tt + 1) * 128], in_=self.psum[:, b, :128])),
                                 reads=["ps%d" % b], writes=["h%d" % dc])
                elif mode == "A":
                    S.dma("sp", self.h_q[:], self.hT[:, t0:t0 + TQ].rearrange("(c p) t -> p c t", p=128), writes=hkeys(), sem="hq")
                    l = layer
                    mix = self.big[:, 0:16, :]
                    S.dma("sp", mix, self.mixT[:, t0:t0 + TQ].rearrange("(c p) t -> p c t", p=128), writes=["big"], sem="big")

                    def epi_add(idx, b, n):
                        S.op("dve", lambda e: e.tensor_tensor(out=self.h_q[:, idx, :], in0=self.psum[:, b, :TQ], in1=self.h_q[:, idx, :], op=ALU.add),
                             reads=["ps%d" % b, "h%d" % idx], writes=["h%d" % idx])
                    self.gemm(2048, [(I["w_out"][l], c * 128, 128) for c in range(DC)], lambda kc: (self.big[:, kc, :], "big"), epi_add)
                    self.norm("ffn_norm", l)
                    j = l // 2
                    if l % 2 == 1:
                        self.router(j)
                        S.dma("sp", self.gTd[:, t0:t0 + TQ], self.gT[:], reads=["gT"], sem="gTst")
                    S.dma("sp", self.hnTd[:, t0:t0 + TQ].rearrange("(c p) t -> p c t", p=128), self.hn_q[:],
                          reads=["hn%d" % k for k in range(DC)], sem="hnst")
                    S.dma("sp", self.hT[:, t0:t0 + TQ].rearrange("(c p) t -> p c t", p=128), self.h_q[:], reads=hkeys(), sem="hq")
                    continue
                if mode == "B":
                    l = layer
                    j = l // 2
                    S.dma("sp", self.h_q[:], self.hT[:, t0:t0 + TQ].rearrange("(c p) t -> p c t", p=128), writes=hkeys(), sem="hq")
                    if l % 2 == 1:
                        S.dma("sp", self.gT[:], self.gTd[:, t0:t0 + TQ], writes=["gT"], sem="gTst")
                    for ee in range(E if l % 2 == 1 else 1):
                        wd = I["moe_w_down"][j, ee] if l % 2 == 1 else I["dense_w_down"][j]
                        S.dma("sp", self.big[:, 0:FC, :], self.actd[ee, :, t0:t0 + TQ].rearrange("(c p) t -> p c t", p=128),
                              writes=["big"], sem="big")
                        self.ffn_down(wd, ee if l % 2 == 1 else None)
                    self.norm("ple_norm", l)
                    for tt in range(TQ // 128):
                        for pc in range(PLE // 128):
                            sl = (tt * 2 + pc) % 2
                            S.dma("sp", self.xin[:, sl, :], I["p"][l, t0 + tt * 128:t0 + (tt + 1) * 128, pc * 128:(pc + 1) * 128],
                                  writes=["xin%d" % sl], sem="xin%d" % sl)
                            b = S.bank()
                            self.transpose_to(self.xin[:, sl, :], "xin%d" % sl, 128, 128, b)
                            S.op("dve", lambda e, pc=pc, tt=tt, b=b: e.tensor_copy(out=self.pT[:, pc, tt * 128:(tt + 1) * 128], in_=self.psum[:, b, :128]),
                                 reads=["ps%d" % b], writes=["pT"])

                    def epi_pp(idx, b, n):
                        S.op("act", lambda e: e.copy(out=pp32[:, idx, :], in_=self.psum[:, b, :TQ]), reads=["ps%d" % b], writes=["big"])
                    self.gemm(PLE, [(I["ple_proj"][l], c * 128, 128) for c in range(DC)], lambda kc: (self.pT[:, kc, :], "pT"), epi_pp)

                    def epi_gate(idx, b, n):
                        sl = idx % 2
                        S.op("act", lambda e: e.activation(out=self.tmp[:, sl, :], in_=self.psum[:, b, :TQ], func=AF.Sigmoid),
                             reads=["ps%d" % b], writes=["tmp%d" % sl])
                        S.op("dve", lambda e: e.tensor_tensor(out=self.tmp[:, sl, :], in0=self.tmp[:, sl, :], in1=pp32[:, idx, :], op=ALU.mult),
                             reads=["tmp%d" % sl, "big"], writes=["tmp%d" % sl])
                        S.op("dve", lambda e: e.tensor_tensor(out=self.h_q[:, idx, :], in0=self.tmp[:, sl, :], in1=self.h_q[:, idx, :], op=ALU.add),
                             reads=["tmp%d" % sl, "h%d" % idx], writes=["h%d" % idx])
                    self.gemm(D, [(I["ple_gate"][l], c * 128, 128) for c in range(DC)], hnrhs, epi_gate)

                nxt = 0 if first else layer + 1
                if nxt < L:
                    S.dma("sp", self.hT[:, t0:t0 + TQ].rearrange("(c p) t -> p c t", p=128), self.h_q[:], reads=hkeys(), sem="hq")
                    self.norm("attn_norm", nxt)
                    w = I["w_in"][nxt]
                    chunks = [(w, c * 128, 128) for c in range(24)] + [(w, 3080 + c * 128, 128) for c in range(32)] \
                        + [(w, 3072, 8), (w, 7176, 8)]

                    def epi_z(idx, b, n, t0=t0):
                        sl = self._stg % 4
                        self._stg += 1
                        eng = self.evac_engine()
                        S.op(eng, lambda e: (e.tensor_copy(out=self.stage[:n, sl, :], in_=self.psum[:n, b, :TQ]) if e is nc.vector
                                             else e.copy(out=self.stage[:n, sl, :], in_=self.psum[:n, b, :TQ])),
                             reads=["ps%d" % b], writes=["stg%d" % sl])
                        r0 = idx * 128 if idx < 56 else (7168 if idx == 56 else 7176)
                        S.dma("sp", self.zT[r0:r0 + n, t0:t0 + TQ], self.stage[:n, sl, :], reads=["stg%d" % sl], sem="stg%d" % sl)
                    self.gemm(D, chunks, hnrhs, epi_z)
                else:
                    def fin(kc):
                        S.op("dve", lambda e: e.scalar_tensor_tensor(out=self.h_q[:, kc, :], in0=self.h_q[:, kc, :], scalar=self.nw[:, kc:kc + 1],
                                                                     in1=self.rstd[:], op0=ALU.mult, op1=ALU.mult),
                             reads=["h%d" % kc, "nw", "rstd"], writes=["h%d" % kc])
                    self.norm("final_norm", None, out_fn=fin)
                    for tt in range(TQ // 128):
                        for dg in range(0, DC, 4):
                            b = S.bank()
                            ng = min(4, DC - dg)
                            for i in range(ng):
                                self.transpose_to(self.h_q[:, dg + i, tt * 128:(tt + 1) * 128], "h%d" % (dg + i), 128, 128, b, col0=i * 128)
                            sl = self._stg % 4
                            self._stg += 1
                            S.op(self.evac_engine(), lambda e, b=b, sl=sl, ng=ng: (e.tensor_copy(out=self.stage[:, sl, :ng * 128], in_=self.psum[:, b, :ng * 128]) if e is nc.vector
                                                                                 else e.copy(out=self.stage[:, sl, :ng * 128], in_=self.psum[:, b, :ng * 128])),
                                 reads=["ps%d" % b], writes=["stg%d" % sl])
                            S.dma("sp", self.out[t0 + tt * 128:t0 + (tt + 1) * 128, dg * 128:(dg + ng) * 128], self.stage[:, sl, :ng * 128],
                                  reads=["stg%d" % sl], sem="stg%d" % sl)
            S.barrier()

    def ffn(self, wg, wu, wd, expert):
        S, cfg = self.S, self.cfg
        TQ, DC, FC, D, FF = cfg.TQ, cfg.DC, cfg.FC, cfg.D, cfg.FF
        chunks = []
        for c in range(0, FC, 2):
            n2 = min(2, FC - c)
            chunks += [(wg, (c + i) * 128, 128) for i in range(n2)] + [(wu, (c + i) * 128, 128) for i in range(n2)]
        order = []
        for c in range(0, FC, 2):
            n2 = min(2, FC - c)
            order += [("g", c + i) for i in range(n2)] + [("u", c + i) for i in range(n2)]
        gbank = {}

        def epi(idx, b, n):
            kind, fc = order[idx]
            if kind == "g":
                sl = fc % 2
                S.op("act", lambda e: e.activation(out=self.tmp[:, sl, :], in_=self.psum[:, b, :TQ], func=AF.Silu),
                     reads=["ps%d" % b], writes=["tmp%d" % sl])
                gbank[fc] = sl
            else:
                sl = gbank.pop(fc)
                S.op("dve", lambda e: e.tensor_tensor(out=self.big[:, fc, :], in0=self.psum[:, b, :TQ], in1=self.tmp[:, sl, :], op=ALU.mult),
                     reads=["ps%d" % b, "tmp%d" % sl], writes=["big"])
        self.gemm(D, chunks, lambda kc: (self.hn_q[:, kc, :], "hn%d" % kc), epi)

        def epi_d(idx, b, n):
            if expert is None:
                S.op("dve", lambda e: e.tensor_tensor(out=self.h_q[:, idx, :], in0=self.psum[:, b, :TQ], in1=self.h_q[:, idx, :], op=ALU.add),
                     reads=["ps%d" % b, "h%d" % idx], writes=["h%d" % idx])
            else:
                sl = idx % 2
                S.op("dve", lambda e: e.tensor_tensor(out=self.tmp[:, sl, :], in0=self.psum[:, b, :TQ], in1=self.gbc[:, expert % 2, :], op=ALU.mult),
                     reads=["ps%d" % b, "gbc%d" % (expert % 2)], writes=["tmp%d" % sl])
                S.op("pool", lambda e: e.tensor_tensor(out=self.h_q[:, idx, :], in0=self.tmp[:, sl, :], in1=self.h_q[:, idx, :], op=ALU.add),
                     reads=["tmp%d" % sl, "h%d" % idx], writes=["h%d" % idx])
        if expert is not None:
            b = S.bank()
            S.op("pe", lambda e: e.matmul(self.psum[:, b, :TQ], lhsT=self.sel[:, expert, :], rhs=self.gT[:], start=True, stop=True),
                 reads=["sel", "gT"], writes=["ps%d" % b])
            S.op("act", lambda e: e.copy(out=self.gbc[:, expert % 2, :], in_=self.psum[:, b, :TQ]), reads=["ps%d" % b], writes=["gbc%d" % (expert % 2)])
        self.gemm(FF, [(wd, c * 128, 128) for c in range(DC)], lambda kc: (self.big[:, kc, :], "big"), epi_d)

    def ffn_down(self, wd, expert):
        S, cfg = self.S, self.cfg
        TQ, DC, FC, D, FF = cfg.TQ, cfg.DC, cfg.FC, cfg.D, cfg.FF

        def epi_d(idx, b, n):
            if expert is None:
                S.op("dve", lambda e: e.tensor_tensor(out=self.h_q[:, idx, :], in0=self.psum[:, b, :TQ], in1=self.h_q[:, idx, :], op=ALU.add),
                     reads=["ps%d" % b, "h%d" % idx], writes=["h%d" % idx])
            else:
                sl = idx % 2
                S.op("dve", lambda e: e.tensor_tensor(out=self.tmp[:, sl, :], in0=self.psum[:, b, :TQ], in1=self.gbc[:, expert % 2, :], op=ALU.mult),
                     reads=["ps%d" % b, "gbc%d" % (expert % 2)], writes=["tmp%d" % sl])
                S.op("pool", lambda e: e.tensor_tensor(out=self.h_q[:, idx, :], in0=self.tmp[:, sl, :], in1=self.h_q[:, idx, :], op=ALU.add),
                     reads=["tmp%d" % sl, "h%d" % idx], writes=["h%d" % idx])
        if expert is not None:
            b = S.bank()
            S.op("pe", lambda e: e.matmul(self.psum[:, b, :TQ], lhsT=self.sel[:, expert, :], rhs=self.gT[:], start=True, stop=True),
                 reads=["sel", "gT"], writes=["ps%d" % b])
            S.op("act", lambda e: e.copy(out=self.gbc[:, expert % 2, :], in_=self.psum[:, b, :TQ]), reads=["ps%d" % b], writes=["gbc%d" % (expert % 2)])
        self.gemm_ksplit(FF, wd, D, lambda kc: (self.big[:, kc, :], "big"), epi_d)

    def ffn_up_phase(self, l):
        S, cfg, I = self.S, self.cfg, self.I
        TQ, DC, FC, D, FF, T, E = cfg.TQ, cfg.DC, cfg.FC, cfg.D, cfg.FF, cfg.T, cfg.E
        NP = T // TQ
        j = l // 2
        with ExitStack() as st:
            self.wslots = self.sb(st, "wslots", [128, NSLOT, SLOT], BF16)
            hn_all = self.sb(st, "hn_all", [128, DC, T], BF16)
            sgbuf = self.sb(st, "sgbuf", [128, 4, T], BF16)
            ast = self.sb(st, "ast", [128, 4, TQ], BF16)
            S.dma("sp", hn_all[:], self.hnTd.rearrange("(c p) t -> p c t", p=128), writes=["hn_all"], sem="hn_all")
            nst = [0]
            for ee in range(E if l % 2 == 1 else 1):
                wg = I["moe_w_gate"][j, ee] if l % 2 == 1 else I["dense_w_gate"][j]
                wu = I["moe_w_up"][j, ee] if l % 2 == 1 else I["dense_w_up"][j]
                chunks, order = [], []
                for c0 in range(0, FC, 4):
                    n4 = min(4, FC - c0)
                    chunks += [(wg, (c0 + i) * 128, 128) for i in range(n4)] + [(wu, (c0 + i) * 128, 128) for i in range(n4)]
                    order += [("g", c0 + i) for i in range(n4)] + [("u", c0 + i) for i in range(n4)]

                def epi(idx, b, n, part, ee=ee, order=order):
                    kind, fc = order[idx]
                    key = "sg%d_%d" % (fc % 4, part)
                    if kind == "g":
                        S.op("act", lambda e: e.activation(out=sgbuf[:, fc % 4, part * TQ:(part + 1) * TQ], in_=self.psum[:, b, :TQ], func=AF.Silu),
                             reads=["ps%d" % b], writes=[key])
                    else:
                        sl = nst[0] % 4
                        nst[0] += 1
                        S.op("dve", lambda e: e.tensor_tensor(out=ast[:, sl, :], in0=self.psum[:, b, :TQ], in1=sgbuf[:, fc % 4, part * TQ:(part + 1) * TQ], op=ALU.mult),
                             reads=["ps%d" % b, key], writes=["ast%d" % sl])
                        S.dma("sp", self.actd[ee, fc * 128:(fc + 1) * 128, part * TQ:(part + 1) * TQ], ast[:, sl, :], reads=["ast%d" % sl], sem="ast%d" % sl)
                self.gemm(D, chunks, lambda kc, part: (hn_all[:, kc, part * TQ:(part + 1) * TQ], "hn_all"), epi, nparts=NP)
            S.barrier()

    def router(self, j):
        S, cfg, I = self.S, self.cfg, self.I
        TQ, DC, E = cfg.TQ, cfg.DC, cfg.E
        S.dma("sp", self.rt32[:], I["router"][j].rearrange("(c p) e -> p c e", p=128), writes=["rt32"], sem="rt32")
        b = S.bank()
        for kc in range(DC):
            sl = kc % 2
            S.op("dve", lambda e, kc=kc, sl=sl: e.scalar_tensor_tensor(out=self.tmp[:, sl, :], in0=self.h_q[:, kc, :], scalar=self.nw[:, kc:kc + 1],
                                                                      in1=self.rstd[:], op0=ALU.mult, op1=ALU.mult),
                 reads=["h%d" % kc, "nw", "rstd"], writes=["tmp%d" % sl])
            S.op("pe", lambda e, kc=kc, sl=sl: e.matmul(self.psum[:E, b, :TQ], lhsT=self.rt32[:, kc, :], rhs=self.tmp[:, sl, :],
                                                        start=(kc == 0), stop=(kc == DC - 1)),
                 reads=["rt32", "tmp%d" % sl], writes=["ps%d" % b] if kc == 0 else [])
        S.last_w["ps%d" % b] = ("e:pe", S.cnt["e:pe"])
        S.op("act", lambda e: e.copy(out=self.lgT[:], in_=self.psum[:E, b, :TQ]), reads=["ps%d" % b], writes=["lgT"])
        NT = TQ // 128
        b2 = S.bank()
        for tt in range(NT):
            self.transpose_to(self.lgT[:, tt * 128:(tt + 1) * 128], "lgT", E, 128, b2, col0=tt * 8)
        W = self.lgw
        S.op("dve", lambda e: e.tensor_copy(out=W[:, 0, :NT * 8], in_=self.psum[:, b2, :NT * 8]), reads=["ps%d" % b2], writes=["lgw"])
        v = lambda k: W[:, k, :NT * 8].rearrange("p (t e) -> p t e", e=8)
        v1 = lambda k: W[:, k, :NT]
        S.op("dve", lambda e: e.tensor_reduce(out=v1(1), in_=v(0), axis=mybir.AxisListType.X, op=ALU.max), reads=["lgw"], writes=["lgw"])
        for tt in range(NT):
            lt = W[:, 0, tt * 8:(tt + 1) * 8]
            m1 = W[:, 1, tt:tt + 1]
            S.op("dve", lambda e, lt=lt, m1=m1, tt=tt: e.tensor_scalar(out=W[:, 2, tt * 8:(tt + 1) * 8], in0=lt, scalar1=m1, scalar2=-1e30,
                                                                      op0=ALU.is_equal, op1=ALU.mult), reads=["lgw"], writes=["lgw"])
            S.op("dve", lambda e, lt=lt, tt=tt: e.tensor_tensor(out=W[:, 2, tt * 8:(tt + 1) * 8], in0=W[:, 2, tt * 8:(tt + 1) * 8], in1=lt, op=ALU.add),
                 reads=["lgw"], writes=["lgw"])
            S.op("dve", lambda e, tt=tt: e.tensor_reduce(out=W[:, 3, tt:tt + 1], in_=W[:, 2, tt * 8:(tt + 1) * 8], axis=mybir.AxisListType.X, op=ALU.max),
                 reads=["lgw"], writes=["lgw"])
            m2 = W[:, 3, tt:tt + 1]
            S.op("dve", lambda e, lt=lt, m2=m2, tt=tt: e.tensor_scalar(out=W[:, 4, tt * 8:(tt + 1) * 8], in0=lt, scalar1=m2, scalar2=None, op0=ALU.is_ge),
                 reads=["lgw"], writes=["lgw"])
            S.op("dve", lambda e, m1=m1, tt=tt: e.tensor_scalar(out=W[:, 5, tt:tt + 1], in0=m1, scalar1=-1.0, scalar2=None, op0=ALU.mult),
                 reads=["lgw"], writes=["lgw"])
            S.op("act", lambda e, lt=lt, tt=tt: e.activation(out=W[:, 6, tt * 8:(tt + 1) * 8], in_=lt, func=AF.Exp, bias=W[:, 5, tt:tt + 1], scale=1.0),
                 reads=["lgw"], writes=["lgw"])
            S.op("act", lambda e, m2=m2, tt=tt: e.activation(out=W[:, 7, tt:tt + 1], in_=m2, func=AF.Exp, bias=W[:, 5, tt:tt + 1], scale=1.0),
                 reads=["lgw"], writes=["lgw"])
            S.op("dve", lambda e, tt=tt: e.tensor_scalar(out=W[:, 7, tt:tt + 1], in0=W[:, 7, tt:tt + 1], scalar1=1.0, scalar2=None, op0=ALU.add),
                 reads=["lgw"], writes=["lgw"])
            S.op("dve", lambda e, tt=tt: e.reciprocal(out=W[:, 7, tt:tt + 1], in_=W[:, 7, tt:tt + 1]), reads=["lgw"], writes=["lgw"])
            S.op("dve", lambda e, tt=tt: e.scalar_tensor_tensor(out=W[:, 4, tt * 8:(tt + 1) * 8], in0=W[:, 6, tt * 8:(tt + 1) * 8], scalar=W[:, 7, tt:tt + 1],
                                                                in1=W[:, 4, tt * 8:(tt + 1) * 8], op0=ALU.mult, op1=ALU.mult),
                 reads=["lgw"], writes=["lgw"])
        b3 = S.bank()
        for tt in range(NT):
            self.transpose_to(W[:, 4, tt * 8:(tt + 1) * 8], "lgw", 128, 8, b3, col0=tt * 128)
        S.op("act", lambda e: e.copy(out=self.gT[:], in_=self.psum[:8, b3, :TQ]), reads=["ps%d" % b3], writes=["gT"])

    def mixer_phase(self, l):
        mixer_phase(self, l)

AX = mybir.AxisListType

def mixer_phase(P, l):
    S, cfg, I, nc = P.S, P.cfg, P.I, P.nc
    T, NCH = cfg.T, cfg.NCH
    lambda_init = 0.8 - 0.6 * math.exp(-0.3 * l)
    ps = P.psum
    ident = P.ident

    def evac(dst, src, rk, wk, eng=None):
        eng = eng or P.evac_engine()
        S.op(eng, lambda e: (e.tensor_copy(out=dst, in_=src) if e is nc.vector else e.copy(out=dst, in_=src)), reads=rk, writes=wk)

    with ExitStack() as st:
        sb = lambda name, shape, dt=F32: P.sb(st, name, shape, dt)
        stg = ExitStack()
        sbg = lambda name, shape, dt=F32: P.sb(stg, name, shape, dt)
        gc = sb("gc", [4, T])
        prm = sb("prm", [4, 8])
        sm = sb("sm", [4, 8, NCH])
        sel4 = sb("sel4", [4, 4, 128])
        ngc = sb("ngc", [4, 4, T])
        ones4 = sb("ones4", [4, 128])
        tok = sb("tok", [128, 6, NCH * 4])
        bc = sb("bc", [128, 2, 4, NCH])
        Gb, Ga, Gi, Gf = sbg("Gb", [4, T]), sbg("Ga", [4, T]), sbg("Gi", [4, T]), sbg("Gf", [4, T])
        cm = sbg("cm", [4, T])
        g1, g2, g3 = sbg("g1", [4, T]), sbg("g2", [4, T]), sbg("g3", [4, T])
        beta, eg, egl = sbg("beta", [4, T]), sbg("eg", [4, T]), sbg("egl", [4, T])
        bb, dd, ww, hden = sbg("bb", [4, T]), sbg("dd", [4, T]), sbg("ww", [4, T]), sbg("hden", [4, T])
        S.dma("sp", Gb[:], P.zT[7168:7172, :], writes=["Gb"], sem="g")
        S.dma("sp", Ga[:], P.zT[7172:7176, :], writes=["Ga"], sem="g")
        S.dma("sp", Gi[:], P.zT[7176:7180, :], writes=["Gi"], sem="g")
        S.dma("sp", Gf[:], P.zT[7180:7184, :], writes=["Gf"], sem="g")
        S.dma("sp", cm[:], I["c_cmask"], writes=["cm"], sem="g")
        for i, nm in enumerate(("dn_a_log", "dn_dt_bias", "ml_i_bias", "ml_f_bias")):
            S.dma("sp", prm[:, i:i + 1], I[nm][l], writes=["prm"], sem="g")
        for k in ("Gb", "Ga", "Gi", "Gf"):
            S.last_w[k] = S.last_w["prm"]
        S.last_w["cm"] = S.last_w["prm"]
        G = ["Gb", "Ga", "Gi", "Gf", "cm", "prm", "g1", "g2", "g3", "beta", "gc", "eg", "egl", "bb", "dd", "ww", "hden", "sm"]

        def gop(eng, fn):
            S.op(eng, fn, reads=G, writes=G)
        gop("dve", lambda e: e.memset(ones4[:], 1.0))
        for h in range(4):
            gop("dve", lambda e, h=h: e.tensor_scalar(out=sel4[:, h, :], in0=ones4[:], scalar1=ident[:4, h:h + 1], scalar2=None, op0=ALU.mult))
        S.last_w["sel4"] = S.last_w["Gb"]
        gop("act", lambda e: e.activation(out=beta[:], in_=Gb[:], func=AF.Sigmoid))
        gop("act", lambda e: e.activation(out=g1[:], in_=Ga[:], func=AF.Exp, bias=prm[:, 1:2], scale=1.0))
        gop("dve", lambda e: e.tensor_scalar(out=g1[:], in0=g1[:], scalar1=1.0, scalar2=None, op0=ALU.add))
        gop("act", lambda e: e.activation(out=g1[:], in_=g1[:], func=AF.Ln))
        gop("act", lambda e: e.activation(out=prm[:, 4:5], in_=prm[:, 0:1], func=AF.Exp))
        gop("dve", lambda e: e.tensor_scalar(out=g1[:], in0=g1[:], scalar1=prm[:, 4:5], scalar2=-1.0, op0=ALU.mult, op1=ALU.mult))
        gop("dve", lambda e: e.tensor_tensor_scan(out=gc[:], data0=cm[:], data1=g1[:], initial=0.0, op0=ALU.mult, op1=ALU.add))
        gop("act", lambda e: e.activation(out=eg[:], in_=gc[:], func=AF.Exp))
        for c in range(NCH):
            last = gc[:, c * 128 + 127:c * 128 + 128]
            gop("dve", lambda e, c=c, last=last: e.tensor_scalar(out=egl[:, c * 128:(c + 1) * 128], in0=gc[:, c * 128:(c + 1) * 128],
                                                                scalar1=-1.0, scalar2=last, op0=ALU.mult, op1=ALU.add))
            gop("act", lambda e, c=c, last=last: e.activation(out=sm[:, 6, c:c + 1], in_=last, func=AF.Exp))
        gop("act", lambda e: e.activation(out=egl[:], in_=egl[:], func=AF.Exp))
        gop("dve", lambda e: e.tensor_scalar(out=g2[:], in0=Gi[:], scalar1=prm[:, 2:3], scalar2=None, op0=ALU.add))
        gop("dve", lambda e: e.tensor_scalar(out=prm[:, 5:6], in0=prm[:, 3:4], scalar1=-1.0, scalar2=None, op0=ALU.mult))
        gop("act", lambda e: e.activation(out=g3[:], in_=Gf[:], func=AF.Exp, bias=prm[:, 5:6], scale=-1.0))
        gop("dve", lambda e: e.tensor_scalar(out=g3[:], in0=g3[:], scalar1=1.0, scalar2=None, op0=ALU.add))
        gop("act", lambda e: e.activation(out=g3[:], in_=g3[:], func=AF.Ln))
        gop("dve", lambda e: e.tensor_scalar(out=g3[:], in0=g3[:], scalar1=-1.0, scalar2=None, op0=ALU.mult))
        gop("dve", lambda e: e.tensor_tensor_scan(out=bb[:], data0=cm[:], data1=g3[:], initial=0.0, op0=ALU.mult, op1=ALU.add))
        gop("dve", lambda e: e.tensor_tensor(out=dd[:], in0=g2[:], in1=bb[:], op=ALU.subtract))
        gop("dve", lambda e: e.tensor_reduce(out=sm[:, 0, :], in_=dd[:].rearrange("p (c t) -> p c t", t=128), axis=AX.X, op=ALU.max))
        gop("dve", lambda e: e.tensor_copy(out=sm[:, 1, :], in_=bb[:].rearrange("p (c t) -> p c t", t=128)[:, :, 127]))
        gop("dve", lambda e: e.tensor_tensor_scan(out=sm[:, 2, :], data0=sm[:, 0, :], data1=sm[:, 1, :], initial=0.0, op0=ALU.max, op1=ALU.add))
        gop("dve", lambda e: e.tensor_tensor(out=sm[:, 3, :], in0=sm[:, 2, :], in1=sm[:, 1, :], op=ALU.subtract))
        gop("dve", lambda e: e.memset(sm[:, 4, :], 0.0))
        if NCH > 1:
            gop("dve", lambda e: e.tensor_copy(out=sm[:, 4, 1:NCH], in_=sm[:, 2, 0:NCH - 1]))
        gop("dve", lambda e: e.tensor_tensor(out=sm[:, 5, :], in0=sm[:, 4, :], in1=sm[:, 3, :], op=ALU.subtract))
        gop("act", lambda e: e.activation(out=sm[:, 5, :], in_=sm[:, 5, :], func=AF.Exp))
        for c in range(NCH):
            Mc = sm[:, 3, c:c + 1]
            gop("dve", lambda e, c=c, Mc=Mc: e.tensor_scalar(out=ww[:, c * 128:(c + 1) * 128], in0=dd[:, c * 128:(c + 1) * 128], scalar1=Mc, scalar2=None, op0=ALU.subtract))
            gop("dve", lambda e, c=c, Mc=Mc: e.tensor_scalar(out=hden[:, c * 128:(c + 1) * 128], in0=bb[:, c * 128:(c + 1) * 128], scalar1=Mc, scalar2=None, op0=ALU.add))
        gop("act", lambda e: e.activation(out=ww[:], in_=ww[:], func=AF.Exp))
        gop("act", lambda e: e.activation(out=hden[:], in_=hden[:], func=AF.Exp, scale=-1.0))
        gop("dve", lambda e: e.tensor_scalar(out=g1[:], in0=beta[:], scalar1=-1.0, scalar2=None, op0=ALU.mult))
        gop("dve", lambda e: e.tensor_scalar(out=g2[:], in0=eg[:], scalar1=-1.0, scalar2=None, op0=ALU.mult))
        gop("dve", lambda e: e.tensor_tensor(out=g3[:], in0=beta[:], in1=egl[:], op=ALU.mult))
        for h in range(4):
            gop("dve", lambda e, h=h: e.tensor_scalar(out=ngc[:, h, :], in0=gc[:], scalar1=ident[:4, h:h + 1], scalar2=-1.0, op0=ALU.mult, op1=ALU.mult))
        S.last_w["ngc"] = S.last_w["Gb"]
        for qi, src in enumerate((g1, g2, g3, eg, ww, hden)):
            b = S.bank()
            for c in range(NCH):
                S.op("pe", lambda e, c=c, b=b, src=src: e.transpose(ps[:, b, c * 4:(c + 1) * 4], src[:, c * 128:(c + 1) * 128], ident[:4, :4]),
                     reads=G + ["ident"], writes=["ps%d" % b] if c == 0 else [])
            S.last_w["ps%d" % b] = ("e:pe", S.cnt["e:pe"])
            evac(tok[:, qi, :], ps[:, b, :NCH * 4], ["ps%d" % b], ["tok"])
        for qi, row in enumerate((6, 5)):
            for h in range(4):
                b = S.bank()
                S.op("pe", lambda e, h=h, b=b, row=row: e.matmul(ps[:, b, :NCH], lhsT=sel4[:, h, :], rhs=sm[:, row, :], start=True, stop=True),
                     reads=G + ["sel4"], writes=["ps%d" % b])
                evac(bc[:, qi, h, :], ps[:, b, :NCH], ["ps%d" % b], ["bc"])

        S.barrier()
        stg.close()
        mstop = ""
        xin = sb("xin_m", [128, T + 3])
        cw = sb("cw", [128, 4])
        qT, kT, vT = sb("qT", [128, T]), sb("kT", [128, T]), sb("vT", [128, T])
        gT_ = sb("gateT", [128, T])
        ktok, vtok = sb("ktok", [128, NCH, 128]), sb("vtok", [128, NCH, 132])
        oT16 = sb("oT16", [128, T], BF16)
        nrm = sb("nrmw", [128, 1])
        sc = sb("sc", [128, 8])
        t1, t2, t3, t4 = sb("t1", [128, 132]), sb("t2", [128, 132]), sb("t3", [128, 132]), sb("t4", [128, 132])
        epsb = sb("epsb_m", [128, 1])
        S.op("dve", lambda e: e.memset(epsb[:], EPS), writes=["epsb_m"])
        S.op("dve", lambda e: e.memset(xin[:, 0:3], 0.0), writes=["xin_m"])

        def load_conv_silu(dst, dkey, row0, cwap, l2=False, scale=None):
            S.dma("sp", xin[:, 3:T + 3], P.zT[row0:row0 + 128, :], writes=["xin_m"], sem="xin_m")
            S.dma("sp", cw[:], cwap, writes=["cw"], sem="cw")
            S.op("dve", lambda e: e.tensor_scalar(out=dst[:], in0=xin[:, 0:T], scalar1=cw[:, 0:1], scalar2=None, op0=ALU.mult),
                 reads=["xin_m", "cw"], writes=[dkey])
            for j in range(1, 4):
                S.op("dve", lambda e, j=j: e.scalar_tensor_tensor(out=dst[:], in0=xin[:, j:T + j], scalar=cw[:, j:j + 1], in1=dst[:], op0=ALU.mult, op1=ALU.add),
                     reads=["xin_m", "cw", dkey], writes=[dkey])
            S.op("act", lambda e: e.activation(out=dst[:], in_=dst[:], func=AF.Silu), reads=[dkey], writes=[dkey])
            if l2:
                for c0 in range(0, T, 512):
                    w_ = min(512, T - c0)
                    S.op("act", lambda e, c0=c0, w_=w_: e.activation(out=xin[:, 3 + c0:3 + c0 + w_], in_=dst[:, c0:c0 + w_], func=AF.Square),
                         reads=[dkey], writes=["xin_m"])
                    b = S.bank()
                    S.op("pe", lambda e, c0=c0, w_=w_, b=b: e.matmul(ps[:, b, :w_], lhsT=P.ones32[:], rhs=xin[:, 3 + c0:3 + c0 + w_], start=True, stop=True),
                         reads=["xin_m", "ones32"], writes=["ps%d" % b])
                    S.op("act", lambda e, c0=c0, w_=w_, b=b: e.activation(out=xin[:, 3 + c0:3 + c0 + w_], in_=ps[:, b, :w_], func=AF.Sqrt, bias=epsb[:], scale=1.0),
                         reads=["ps%d" % b, "epsb_m"], writes=["xin_m"])
                    S.op("dve", lambda e, c0=c0, w_=w_: e.reciprocal(out=xin[:, 3 + c0:3 + c0 + w_], in_=xin[:, 3 + c0:3 + c0 + w_]), reads=["xin_m"], writes=["xin_m"])
                    if scale is None:
                        S.op("dve", lambda e, c0=c0, w_=w_: e.tensor_tensor(out=dst[:, c0:c0 + w_], in0=dst[:, c0:c0 + w_], in1=xin[:, 3 + c0:3 + c0 + w_], op=ALU.mult),
                             reads=["xin_m", dkey], writes=[dkey])
                    else:
                        S.op("dve", lambda e, c0=c0, w_=w_: e.scalar_tensor_tensor(out=dst[:, c0:c0 + w_], in0=dst[:, c0:c0 + w_], scalar=scale, in1=xin[:, 3 + c0:3 + c0 + w_],
                                                                                 op0=ALU.mult, op1=ALU.mult),
                             reads=["xin_m", dkey], writes=[dkey])

        def to_tok(src, skey, dst, dkey, width=128):
            for c in range(NCH):
                b = S.bank()
                P.transpose_to(src[:, c * 128:(c + 1) * 128], skey, 128, 128, b)
                evac(dst[:, c, 0:128], ps[:, b, :128], ["ps%d" % b], [dkey])

        obuf = sb("obuf", [128, NCH, 128])
        osq = sb("osq", [128, NCH, 128])
        rs = sb("rs", [128, NCH])

        def finish_all(gate_T, extra):
            S.op("act", lambda e: e.activation(out=osq[:], in_=obuf[:], func=AF.Square), reads=["obuf"], writes=["osq"])
            S.op("dve", lambda e: e.tensor_reduce(out=rs[:], in_=osq[:], axis=AX.X, op=ALU.add), reads=["osq"], writes=["rs"])
            S.op("act", lambda e: e.activation(out=rs[:], in_=rs[:], func=AF.Sqrt, bias=epsb[:], scale=1.0 / 128), reads=["rs", "epsb_m"], writes=["rs"])
            S.op("dve", lambda e: e.reciprocal(out=rs[:], in_=rs[:]), reads=["rs"], writes=["rs"])
            for c in range(NCH):
                S.op("dve" if c % 2 == 0 else "pool", lambda e, c=c: e.tensor_scalar(out=osq[:, c, :], in0=obuf[:, c, :], scalar1=rs[:, c:c + 1], scalar2=float(extra), op0=ALU.mult, op1=ALU.mult),
                     reads=["obuf", "rs"], writes=["osq%d" % c])
            for c0 in range(0, NCH, 4):
                nn = min(4, NCH - c0)
                b = S.bank()
                for i in range(nn):
                    S.op("pe", lambda e, i=i, b=b, c0=c0: e.transpose(ps[:, b, i * 128:(i + 1) * 128], osq[:, c0 + i, :], ident[:, :]),
                         reads=["osq%d" % (c0 + i), "osq", "ident"], writes=["ps%d" % b] if i == 0 else [])
                S.last_w["ps%d" % b] = ("e:pe", S.cnt["e:pe"])
                if gate_T is not None:
                    S.op("dve", lambda e, b=b, c0=c0, nn=nn: e.scalar_tensor_tensor(out=oT16[:, c0 * 128:(c0 + nn) * 128], in0=ps[:, b, :nn * 128], scalar=nrm[:, 0:1],
                                                                                 in1=gate_T[:, c0 * 128:(c0 + nn) * 128], op0=ALU.mult, op1=ALU.mult),
                         reads=["ps%d" % b, "nrmw", "gateT"], writes=["oT16"])
                else:
                    S.op("dve", lambda e, b=b, c0=c0, nn=nn: e.tensor_scalar(out=oT16[:, c0 * 128:(c0 + nn) * 128], in0=ps[:, b, :nn * 128], scalar1=nrm[:, 0:1], scalar2=None, op0=ALU.mult),
                         reads=["ps%d" % b, "nrmw"], writes=["oT16"])
            for c in range(NCH):
                for tkn in S.readers.get("osq%d" % c, []):
                    S.readers.setdefault("osq", []).append(tkn)

        def finish_head(otok, okey, c, wkey_scale, gate_ap, row0, extra=1.0):
            S.op("act", lambda e: e.activation(out=t4[:, 0:128], in_=otok, func=AF.Square), reads=[okey], writes=["t4"])
            S.op("dve", lambda e: e.tensor_reduce(out=sc[:, 0:1], in_=t4[:, 0:128], axis=AX.X, op=ALU.add), reads=["t4"], writes=["sc"])
            S.op("act", lambda e: e.activation(out=sc[:, 0:1], in_=sc[:, 0:1], func=AF.Sqrt, bias=epsb[:], scale=1.0 / 128), reads=["sc", "epsb_m"], writes=["sc"])
            S.op("dve", lambda e: e.reciprocal(out=sc[:, 0:1], in_=sc[:, 0:1]), reads=["sc"], writes=["sc"])
            S.op("dve", lambda e: e.tensor_scalar(out=t4[:, 0:128], in0=otok, scalar1=sc[:, 0:1], scalar2=extra, op0=ALU.mult, op1=ALU.mult),
                 reads=[okey, "sc"], writes=["t4"])
            b = S.bank()
            P.transpose_to(t4[:, 0:128], "t4", 128, 128, b)
            if gate_ap is not None:
                S.op("dve", lambda e: e.scalar_tensor_tensor(out=oT16[:, c * 128:(c + 1) * 128], in0=ps[:, b, :128], scalar=nrm[:, 0:1], in1=gate_ap,
                                                             op0=ALU.mult, op1=ALU.mult), reads=["ps%d" % b, "nrmw", "gateT"], writes=["oT16"])
            else:
                S.op("dve", lambda e: e.tensor_scalar(out=oT16[:, c * 128:(c + 1) * 128], in0=ps[:, b, :128], scalar1=nrm[:, 0:1], scalar2=None, op0=ALU.mult),
                     reads=["ps%d" % b, "nrmw"], writes=["oT16"])

        with ExitStack() as st2:
            sb2 = lambda name, shape, dt=F32: P.sb(st2, name, shape, dt)
            attnT = sb2("attnT", [128, NCH, 128])
            RT = sb2("RT", [128, NCH, 128])
            E_, Ei, Es = sb2("E_", [128, 128]), sb2("Ei", [128, 128]), sb2("Es", [128, 128])
            Pm, Qm, Rm = sb2("Pm", [128, 2, 128]), sb2("Qm", [128, 2, 128]), sb2("Rm", [128, 2, 128])
            St = sb2("St", [128, 128])
            S.dma("sp", nrm[:], I["dn_norm"][l], writes=["nrmw"], sem="nrmw")
            for h in range(NH_):
                load_conv_silu(qT, "qT", h * 128, I["conv_dn"][l, h * 128:(h + 1) * 128, :], l2=True, scale=128.0 ** -0.5)
                load_conv_silu(kT, "kT", 512 + h * 128, I["conv_dn"][l, 512 + h * 128:512 + (h + 1) * 128, :], l2=True)
                load_conv_silu(vT, "vT", 1024 + h * 128, I["conv_dn"][l, 1024 + h * 128:1024 + (h + 1) * 128, :])
                S.dma("sp", gT_[:], P.zT[2560 + h * 128:2560 + (h + 1) * 128, :], writes=["gateT"], sem="gateT")
                S.op("act", lambda e: e.activation(out=gT_[:], in_=gT_[:], func=AF.Silu), reads=["gateT"], writes=["gateT"])
                to_tok(kT, "kT", ktok, "ktok")
                to_tok(vT, "vT", vtok, "vtok")
                if mstop == "dnload":
                    continue
                for c in range(NCH):
                    cs = slice(c * 128, (c + 1) * 128)
                    col = c * 4 + h
                    bD = S.bank()
                    def mmD(e, bD=bD, cs=cs, h=h):
                        e.matmul(ps[:, bD, :128], lhsT=sel4[:, h, :], rhs=gc[:, cs], start=True, stop=False)
                        return e.matmul(ps[:, bD, :128], lhsT=ngc[:, h, cs], rhs=ones4[:], start=False, stop=True)
                    S.op("pe", mmD, reads=G + ["sel4", "ngc"], writes=["ps%d" % bD])
                    S.op("dve", lambda e, bD=bD: e.tensor_scalar(out=E_[:], in0=ps[:, bD, :128], scalar1=0.0, scalar2=None, op0=ALU.min), reads=["ps%d" % bD], writes=["E_"])
                    S.op("act", lambda e: e.activation(out=E_[:], in_=E_[:], func=AF.Exp), reads=["E_"], writes=["E_"])
                    S.op("pool", lambda e: e.tensor_tensor(out=Ei[:], in0=E_[:], in1=P.mui[:], op=ALU.mult), reads=["E_", "mui"], writes=["Ei"])
                    S.op("pool", lambda e: e.tensor_tensor(out=Es[:], in0=E_[:], in1=P.mus[:], op=ALU.mult), reads=["E_", "mus"], writes=["Es"])
                    bK = S.bank()
                    S.op("pe", lambda e, bK=bK, cs=cs: e.matmul(ps[:, bK, :128], lhsT=kT[:, cs], rhs=qT[:, cs], start=True, stop=True), reads=["kT", "qT"], writes=["ps%d" % bK])
                    S.op("dve", lambda e, bK=bK, c=c: e.tensor_tensor(out=attnT[:, c, :], in0=ps[:, bK, :128], in1=Ei[:], op=ALU.mult), reads=["ps%d" % bK, "Ei"], writes=["attnT"])
                    bK2 = S.bank()
                    S.op("pe", lambda e, bK2=bK2, cs=cs: e.matmul(ps[:, bK2, :128], lhsT=kT[:, cs], rhs=kT[:, cs], start=True, stop=True), reads=["kT"], writes=["ps%d" % bK2])
                    S.op("dve", lambda e, bK2=bK2, col=col: e.scalar_tensor_tensor(out=Qm[:, 0, :], in0=ps[:, bK2, :128], scalar=tok[:, 0, col:col + 1], in1=Es[:],
                                                                                  op0=ALU.mult, op1=ALU.mult), reads=["ps%d" % bK2, "tok", "Es"], writes=["Q0"])
                    bT = S.bank()
                    P.transpose_to(Qm[:, 0, :], "Q0", 128, 128, bT)
                    evac(Pm[:, 0, :], ps[:, bT, :128], ["ps%d" % bT], ["P0"])
                    S.op("dve", lambda e: e.tensor_tensor(out=Rm[:, 0, :], in0=Qm[:, 0, :], in1=ident[:], op=ALU.add), reads=["Q0", "ident"], writes=["R0"])
                    cur = 0
                    for m in range(1, 8):
                        nx = 1 - cur
                        b1 = S.bank()
                        S.op("pe", lambda e, b1=b1, cur=cur: e.matmul(ps[:, b1, :128], lhsT=Qm[:, cur, :], rhs=Pm[:, cur, :], start=True, stop=True),
                             reads=["Q%d" % cur, "P%d" % cur], writes=["ps%d" % b1])
                        if m <= 6:
                            b2 = S.bank()
                            S.op("pe", lambda e, b2=b2, cur=cur: e.matmul(ps[:, b2, :128], lhsT=Pm[:, cur, :], rhs=Qm[:, cur, :], start=True, stop=True),
                                 reads=["Q%d" % cur, "P%d" % cur], writes=["ps%d" % b2])
                        evac(Pm[:, nx, :], ps[:, b1, :128], ["ps%d" % b1], ["P%d" % nx])
                        if m <= 6:
                            evac(Qm[:, nx, :], ps[:, b2, :128], ["ps%d" % b2], ["Q%d" % nx])
                        b3 = S.bank()
                        S.op("pe", lambda e, b3=b3, cur=cur, nx=nx: e.matmul(ps[:, b3, :128], lhsT=Pm[:, nx, :], rhs=Rm[:, cur, :], start=True, stop=True),
                             reads=["P%d" % nx, "R%d" % cur], writes=["ps%d" % b3])
                        dst = RT[:, c, :] if m == 7 else Rm[:, nx, :]
                        S.op("dve", lambda e, b3=b3, cur=cur, dst=dst: e.tensor_tensor(out=dst, in0=ps[:, b3, :128], in1=Rm[:, cur, :], op=ALU.add),
                             reads=["ps%d" % b3, "R%d" % cur], writes=["RT"] if m == 7 else ["R%d" % nx])
                        cur = nx
                if mstop == "dnpre":
                    continue
                S.op("dve", lambda e: e.memset(St[:], 0.0), writes=["St"])
                for c in range(NCH):
                    cs = slice(c * 128, (c + 1) * 128)
                    col = c * 4 + h
                    b1 = S.bank()
                    S.op("pe", lambda e, b1=b1, cs=cs: e.matmul(ps[:, b1, :128], lhsT=kT[:, cs], rhs=St[:], start=True, stop=True), reads=["kT", "St"], writes=["ps%d" % b1])
                    S.op("dve", lambda e, b1=b1, c=c, col=col: e.scalar_tensor_tensor(out=t1[:, 0:128], in0=ps[:, b1, :128], scalar=tok[:, 1, col:col + 1], in1=vtok[:, c, 0:128],
                                                                                     op0=ALU.mult, op1=ALU.add), reads=["ps%d" % b1, "tok", "vtok"], writes=["t1"])
                    b2 = S.bank()
                    S.op("pe", lambda e, b2=b2, c=c: e.matmul(ps[:, b2, :128], lhsT=RT[:, c, :], rhs=t1[:, 0:128], start=True, stop=True), reads=["RT", "t1"], writes=["ps%d" % b2])
                    S.op("dve", lambda e, b2=b2, col=col: e.tensor_scalar(out=t2[:, 0:128], in0=ps[:, b2, :128], scalar1=tok[:, 0, col:col + 1], scalar2=-1.0, op0=ALU.mult, op1=ALU.mult),
                         reads=["ps%d" % b2, "tok"], writes=["t2"])
                    S.op("dve", lambda e, b2=b2, col=col: e.tensor_scalar(out=t3[:, 0:128], in0=ps[:, b2, :128], scalar1=tok[:, 2, col:col + 1], scalar2=None, op0=ALU.mult),
                         reads=["ps%d" % b2, "tok"], writes=["t3"])
                    b3 = S.bank()
                    S.op("pe", lambda e, b3=b3, cs=cs: e.matmul(ps[:, b3, :128], lhsT=qT[:, cs], rhs=St[:], start=True, stop=True), reads=["qT", "St"], writes=["ps%d" % b3])
                    b4 = S.bank()
                    S.op("pe", lambda e, b4=b4, c=c: e.matmul(ps[:, b4, :128], lhsT=attnT[:, c, :], rhs=t2[:, 0:128], start=True, stop=True), reads=["attnT", "t2"], writes=["ps%d" % b4])
                    b5 = S.bank()
                    S.op("pe", lambda e, b5=b5, c=c: e.matmul(ps[:, b5, :128], lhsT=ktok[:, c, :], rhs=t3[:, 0:128], start=True, stop=True), reads=["ktok", "t3"], writes=["ps%d" % b5])
                    S.op("dve", lambda e, b5=b5, c=c, h=h: e.scalar_tensor_tensor(out=St[:], in0=St[:], scalar=bc[:, 0, h, c:c + 1], in1=ps[:, b5, :128], op0=ALU.mult, op1=ALU.add),
                         reads=["St", "bc", "ps%d" % b5], writes=["St"])
                    evac(t1[:, 0:128], ps[:, b4, :128], ["ps%d" % b4], ["t1"], eng="act")
                    S.op("dve", lambda e, b3=b3, col=col, c=c: e.scalar_tensor_tensor(out=obuf[:, c, :], in0=ps[:, b3, :128], scalar=tok[:, 3, col:col + 1], in1=t1[:, 0:128],
                                                                                 op0=ALU.mult, op1=ALU.add), reads=["ps%d" % b3, "tok", "t1"], writes=["obuf"])
                finish_all(gT_, 1.0)
                S.dma("sp", P.mixT[h * 128:(h + 1) * 128, :], oT16[:], reads=["oT16"], sem="oT16")

        if mstop in ("dn", "dnload", "dnpre"):
            S.barrier()
            return
        with ExitStack() as st2:
            sb2 = lambda name, shape, dt=F32: P.sb(st2, name, shape, dt)
            stp = sb2("stp", [128, 132])
            sT = sb2("sT", [128, 128])
            muis = sb2("muis", [128, 128])
            S.op("dve", lambda e: e.tensor_scalar(out=muis[:], in0=P.mui[:], scalar1=128.0 ** -0.5, scalar2=None, op0=ALU.mult), reads=["mui"], writes=["muis"])
            S.dma("sp", nrm[:], I["ml_norm"][l], reads=[], writes=["nrmw"], sem="nrmw")
            for h in range(NH_):
                load_conv_silu(qT, "qT", 1536 + h * 128, I["conv_ml"][l, h * 128:(h + 1) * 128, :])
                load_conv_silu(kT, "kT", 2048 + h * 128, I["conv_ml"][l, 512 + h * 128:512 + (h + 1) * 128, :])
                S.dma("sp", vT[:], P.zT[6144 + h * 128:6144 + (h + 1) * 128, :], writes=["vT"], sem="vT")
                S.dma("sp", gT_[:], P.zT[6656 + h * 128:6656 + (h + 1) * 128, :], writes=["gateT"], sem="gateT")
                S.op("act", lambda e: e.activation(out=gT_[:], in_=gT_[:], func=AF.Sigmoid), reads=["gateT"], writes=["gateT"])
                to_tok(kT, "kT", ktok, "ktok")
                to_tok(vT, "vT", vtok, "vtok")
                S.op("dve", lambda e: e.memset(vtok[:, :, 128:129], 1.0), reads=[], writes=["vtok"])
                S.op("dve", lambda e: e.memset(stp[:], 0.0), writes=["stp"])
                for c in range(NCH):
                    cs = slice(c * 128, (c + 1) * 128)
                    col = c * 4 + h
                    b1 = S.bank()
                    S.op("pe", lambda e, b1=b1, cs=cs: e.matmul(ps[:, b1, :128], lhsT=kT[:, cs], rhs=qT[:, cs], start=True, stop=True), reads=["kT", "qT"], writes=["ps%d" % b1])
                    S.op("dve", lambda e, b1=b1, col=col: e.scalar_tensor_tensor(out=sT[:], in0=ps[:, b1, :128], scalar=tok[:, 4, col:col + 1], in1=muis[:], op0=ALU.mult, op1=ALU.mult),
                         reads=["ps%d" % b1, "tok", "muis"], writes=["sT"])
                    b2 = S.bank()
                    def mm2(e, b2=b2, c=c, cs=cs):
                        e.matmul(ps[:, b2, :129], lhsT=sT[:], rhs=vtok[:, c, 0:129], start=True, stop=False)
                        return e.matmul(ps[:, b2, :129], lhsT=qT[:, cs], rhs=stp[:, 0:129], start=False, stop=True)
                    S.op("pe", mm2, reads=["sT", "vtok", "qT", "stp"], writes=["ps%d" % b2])
                    S.op("dve", lambda e, c=c, col=col: e.tensor_scalar(out=t1[:, 0:129], in0=vtok[:, c, 0:129], scalar1=tok[:, 4, col:col + 1], scalar2=128.0 ** -0.5, op0=ALU.mult, op1=ALU.mult),
                         reads=["vtok", "tok"], writes=["t1"])
                    b3 = S.bank()
                    S.op("pe", lambda e, b3=b3, c=c: e.matmul(ps[:, b3, :129], lhsT=ktok[:, c, :], rhs=t1[:, 0:129], start=True, stop=True), reads=["ktok", "t1"], writes=["ps%d" % b3])
                    if c + 1 < NCH:
                        S.op("dve", lambda e, b3=b3: e.tensor_tensor(out=stp[:, 0:129], in0=ps[:, b3, :129], in1=stp[:, 0:129], op=ALU.add), reads=["ps%d" % b3, "stp"], writes=["stp"])
                        S.op("dve", lambda e, c=c, h=h: e.tensor_scalar(out=stp[:, 0:129], in0=stp[:, 0:129], scalar1=bc[:, 1, h, c + 1:c + 2], scalar2=None, op0=ALU.mult),
                             reads=["stp", "bc"], writes=["stp"])
                    S.op("act", lambda e, b2=b2, col=col: e.activation(out=sc[:, 1:2], in_=ps[:, b2, 128:129], func=AF.Abs),
                         reads=["ps%d" % b2, "tok"], writes=["sc1"])
                    S.op("dve", lambda e, col=col: e.tensor_tensor(out=sc[:, 1:2], in0=sc[:, 1:2], in1=tok[:, 5, col:col + 1], op=ALU.max),
                         reads=["sc1", "tok"], writes=["sc1"])
                    S.op("dve", lambda e: e.reciprocal(out=sc[:, 1:2], in_=sc[:, 1:2]), reads=["sc1"], writes=["sc1"])
                    S.op("dve", lambda e, b2=b2, c=c: e.tensor_scalar(out=obuf[:, c, :], in0=ps[:, b2, 0:128], scalar1=sc[:, 1:2], scalar2=None, op0=ALU.mult), reads=["ps%d" % b2, "sc1"], writes=["obuf"])
                finish_all(gT_, 1.0)
                S.dma("sp", P.mixT[1536 + h * 128:1536 + (h + 1) * 128, :], oT16[:], reads=["oT16"], sem="oT16")

        if mstop == "ml":
            S.barrier()
            return
        with ExitStack() as st2:
            sb2 = lambda name, shape, dt=F32: P.sb(st2, name, shape, dt)
            QG = cfg.QG
            NG = T // QG
            NS = QG // 128
            qa = sb2("qa", [67, 2, T], BF16)
            ka = sb2("ka", [67, 2, T], BF16)
            v1 = sb2("v1", [128, NCH, 132], BF16)
            PT = sb2("PT", [128, NCH, QG], BF16)
            om = sb2("om", [128, 2, NS, 128])
            lam = sb2("lam", [128, 8])
            lq = sb2("lq", [128, 4, 64])
            for i, nm in enumerate(("diff_lq1", "diff_lk1", "diff_lq2", "diff_lk2")):
                S.dma("sp", lq[:, i, :], I[nm][l:l + 1, :].broadcast_to([128, 64]),
                      writes=["lq"], sem="lq")
            S.last_w["lq"] = (S.dsem("lq"), S.cnt[S.dsem("lq")])
            S.op("dve", lambda e: e.tensor_tensor(out=lq[:, 0, :], in0=lq[:, 0, :], in1=lq[:, 1, :], op=ALU.mult), reads=["lq"], writes=["lq"])
            S.op("dve", lambda e: e.tensor_tensor(out=lq[:, 2, :], in0=lq[:, 2, :], in1=lq[:, 3, :], op=ALU.mult), reads=["lq"], writes=["lq"])
            S.op("dve", lambda e: e.tensor_reduce(out=lam[:, 0:1], in_=lq[:, 0, :], axis=AX.X, op=ALU.add), reads=["lq"], writes=["lam"])
            S.op("dve", lambda e: e.tensor_reduce(out=lam[:, 1:2], in_=lq[:, 2, :], axis=AX.X, op=ALU.add), reads=["lq"], writes=["lam"])
            S.op("act", lambda e: e.activation(out=lam[:, 0:2], in_=lam[:, 0:2], func=AF.Exp), reads=["lam"], writes=["lam"])
            S.op("dve", lambda e: e.tensor_tensor(out=lam[:, 2:3], in0=lam[:, 1:2], in1=lam[:, 0:1], op=ALU.subtract), reads=["lam"], writes=["lam"])
            S.op("dve", lambda e: e.tensor_scalar(out=lam[:, 2:3], in0=lam[:, 2:3], scalar1=-lambda_init, scalar2=None, op0=ALU.add), reads=["lam"], writes=["lam"])
            S.dma("sp", nrm[:], I["diff_norm"][l], writes=["nrmw"], sem="nrmw")
            for h in range(8):
                slope = 2.0 ** (-8.0 * (h + 1) / 8)
                for m in range(2):
                    r = h * 128 + m * 64
                    S.dma("sp", xin[0:64, 3:T + 3], P.zT[3072 + r:3072 + r + 64, :], writes=["xin_m"], sem="xin_m")
                    S.op("act", lambda e, m=m: e.activation(out=qa[0:64, m, :], in_=xin[0:64, 3:T + 3], func=AF.Copy, scale=0.125), reads=["xin_m"], writes=["qa"])
                    S.dma("sp", xin[0:64, 3:T + 3], P.zT[4096 + r:4096 + r + 64, :], writes=["xin_m"], sem="xin_m")
                    S.op("act", lambda e, m=m: e.activation(out=ka[0:64, m, :], in_=xin[0:64, 3:T + 3], func=AF.Copy), reads=["xin_m"], writes=["ka"])
                    S.dma("pool", qa[64:67, m, :], I["c_qaug"][h], writes=["qa"], sem="qa")
                    S.dma("pool", ka[64:67, m, :], I["c_kaug"][h], writes=["ka"], sem="ka")
                S.dma("sp", vT[:], P.zT[5120 + h * 128:5120 + (h + 1) * 128, :], writes=["vT"], sem="vT")
                for c in range(NCH):
                    b = S.bank()
                    P.transpose_to(vT[:, c * 128:(c + 1) * 128], "vT", 128, 128, b)
                    evac(v1[:, c, 0:128], ps[:, b, :128], ["ps%d" % b], ["v1"])
                S.op("dve", lambda e: e.memset(v1[:, :, 128:129], 1.0), writes=["v1"])
                for g in range(NG):
                    nJ = (g + 1) * NS
                    for m in range(2):
                        for J in range(nJ):
                            r = J - g * NS
                            col0 = max(r, 0) * 128
                            ncol = QG - col0
                            cGJ = -slope * (g * QG - J * 128)
                            b = S.bank()
                            S.op("pe", lambda e, b=b, J=J, m=m, g=g, col0=col0, ncol=ncol: e.matmul(ps[:, b, :ncol], lhsT=ka[:, m, J * 128:(J + 1) * 128],
                                                                                                 rhs=qa[:, m, g * QG + col0:(g + 1) * QG], start=True, stop=True),
                                 reads=["qa", "ka"], writes=["ps%d" % b])
                            S.op("act", lambda e, b=b, J=J, col0=col0, ncol=ncol, cGJ=cGJ: e.activation(out=PT[:, J, col0:QG], in_=ps[:, b, :ncol], func=AF.Exp, bias=float(cGJ), scale=1.0),
                                 reads=["ps%d" % b], writes=["PT%d" % J])
                            if r >= 0:
                                S.op("pool", lambda e, J=J, col0=col0: e.tensor_tensor(out=PT[:, J, col0:col0 + 128], in0=PT[:, J, col0:col0 + 128], in1=P.mui16[:], op=ALU.mult),
                                     reads=["PT%d" % J, "mui16"], writes=["PT%d" % J])
                        for i in range(NS):
                            nK = g * NS + i + 1
                            b = S.bank()
                            def mmpv(e, b=b, i=i, nK=nK):
                                ins = None
                                for J in range(nK):
                                    ins = e.matmul(ps[:, b, :129], lhsT=PT[:, J, i * 128:(i + 1) * 128], rhs=v1[:, J, 0:129], start=(J == 0), stop=(J == nK - 1))
                                return ins
                            S.op("pe", mmpv, reads=["PT%d" % J for J in range(nK)] + ["v1"], writes=["ps%d" % b])
                            S.op("dve", lambda e, b=b: e.reciprocal(out=sc[:, 2:3], in_=ps[:, b, 128:129]), reads=["ps%d" % b], writes=["sc2"])
                            S.op("dve", lambda e, b=b, m=m, i=i: e.tensor_scalar(out=om[:, m, i, :], in0=ps[:, b, 0:128], scalar1=sc[:, 2:3], scalar2=None, op0=ALU.mult),
                                 reads=["ps%d" % b, "sc2"], writes=["om%d_%d" % (m, i)])
                    for i in range(NS):
                        S.op("dve", lambda e, i=i, g=g: e.scalar_tensor_tensor(out=obuf[:, g * NS + i, :], in0=om[:, 1, i, :], scalar=lam[:, 2:3], in1=om[:, 0, i, :], op0=ALU.mult, op1=ALU.add),
                             reads=["om0_%d" % i, "om1_%d" % i, "lam"], writes=["obuf"])
                finish_all(None, 1.0 - lambda_init)
                S.dma("sp", P.mixT[512 + h * 128:512 + (h + 1) * 128, :], oT16[:], reads=["oT16"], sem="oT16")
        S.barrier()


NH_ = 4
_bias_cache = {}


def P_bias(P, S, val):
    return P.cbias_ap(val)


def _consts(T):
    j = np.arange(128)[:, None]
    i = np.arange(128)[None, :]
    c = {}
    c["c_ident"] = np.eye(128, dtype=np.float32)
    c["c_mui"] = (i >= j).astype(np.float32)
    c["c_mus"] = (i > j).astype(np.float32)
    cm = np.ones((4, T), np.float32)
    cm[:, ::128] = 0
    c["c_cmask"] = cm
    QG = min(512, T)
    pos = np.arange(T)
    qa = np.zeros((8, 3, T), np.float32)
    ka = np.zeros((8, 3, T), np.float32)
    for h in range(8):
        s = 2.0 ** (-8.0 * (h + 1) / 8)
        a = pos % QG
        qa[h, 0] = -s * (a % 128)
        qa[h, 1] = -s * 128 * (a // 128)
        qa[h, 2] = 1.0
        ka[h, 0] = 1.0
        ka[h, 1] = 1.0
        ka[h, 2] = s * (pos % 128)
    c["c_qaug"] = qa
    c["c_kaug"] = ka
    return c


def _make_maps(inp, cfg, ncores):
    L = cfg.L
    cst = _consts(cfg.T)
    sh = {}
    sh["conv_dn"] = np.ascontiguousarray(np.transpose(inp["conv_dn"], (0, 2, 1)))
    sh["conv_ml"] = np.ascontiguousarray(np.transpose(inp["conv_ml"], (0, 2, 1)))
    for nm in ("dn_a_log", "dn_dt_bias", "ml_i_bias", "ml_f_bias", "dn_norm", "diff_norm", "ml_norm"):
        sh[nm] = np.ascontiguousarray(inp[nm][..., None])
    for nm in ("attn_norm", "ffn_norm", "ple_norm"):
        sh[nm] = np.ascontiguousarray(inp[nm].reshape(L, -1, 128).transpose(0, 2, 1))
    sh["final_norm"] = np.ascontiguousarray(inp["final_norm"].reshape(-1, 128).T)
    base = {}
    for k, v in inp.items():
        if k in ("x", "p"):
            continue
        base[k] = sh[k] if k in sh else np.ascontiguousarray(v)
    base.update(cst)
    maps = []
    for b in range(ncores):
        m = dict(base)
        m["x"] = np.ascontiguousarray(inp["x"][b])
        m["p"] = np.ascontiguousarray(inp["p"][:, b])
        maps.append(m)
    return maps


def kernel(**inputs):
    inp = {k: np.asarray(v, dtype=np.float32) for k, v in inputs.items()}
    B, T, D = inp["x"].shape
    L = inp["w_in"].shape[0]
    FF = inp["dense_w_gate"].shape[2]
    cfg = Cfg(D=D, T=T, FF=FF, L=L, TQ=min(512, T))
    prog = Prog(cfg)
    nc = prog.build()
    maps = _make_maps(inp, cfg, B)
    res = run_bass_kernel_spmd(nc, maps, core_ids=list(range(B)))
    return np.stack([np.asarray(r["out"], dtype=np.float32) for r in res.results])
```

```python
import math
from contextlib import ExitStack
import numpy as np
import concourse.bass as bass
import concourse.mybir as mybir
from concourse.bass_utils import run_bass_kernel_spmd

F32 = mybir.dt.float32
BF16 = mybir.dt.bfloat16
AF = mybir.ActivationFunctionType
ALU = mybir.AluOpType
EPS = 1e-6
NH = 4
NDH = 8
SLOT = 8192
NSLOT = 3


class Cfg:
    def __init__(self, D=2048, T=2048, FF=7168, E=8, PLE=256, L=2, TQ=512):
        self.D, self.T, self.FF, self.E, self.PLE, self.L, self.TQ = D, T, FF, E, PLE, L, TQ
        self.DC = D // 128
        self.FC = FF // 128
        self.NQ = T // TQ
        self.NCH = T // 128
        self.QG = min(512, T)
        self.NIN = 7184


class Sched:
    def __init__(self, nc, es):
        self.nc = nc
        self.es = es
        self.eng = {"pe": nc.tensor, "dve": nc.vector, "act": nc.scalar, "pool": nc.gpsimd, "sp": nc.sync}
        self.sem = {}
        self.cnt = {}
        for e in self.eng:
            self.sem["e:" + e] = es.enter_context(nc.semaphore("sem_" + e))
            self.cnt["e:" + e] = 0
        self.seen = {}
        self.last_w = {}
        self.readers = {}
        self.nbank = 0
        self.nslot = 0
        self.ndsem = 0

    def dsem(self, name):
        k = "d:" + name
        if k not in self.sem:
            self.sem[k] = self.es.enter_context(self.nc.semaphore("dsem_%d" % self.ndsem))
            self.ndsem += 1
            self.cnt[k] = 0
        return k

    def _deps(self, reads, writes):
        deps = {}
        def add(tok):
            if tok is None:
                return
            k, v = tok
            if deps.get(k, 0) < v:
                deps[k] = v
        for k in reads:
            add(self.last_w.get(k))
        for k in writes:
            add(self.last_w.get(k))
            for t in self.readers.get(k, ()):
                add(t)
        return deps

    def _wait(self, e, deps):
        eng = self.eng[e]
        for k, v in deps.items():
            if k == "e:" + e and e == "pe":
                continue
            if self.seen.get((e, k), 0) >= v:
                continue
            eng.wait_ge(self.sem[k], v)
            self.seen[(e, k)] = v

    def _record(self, tok, reads, writes):
        for k in writes:
            self.last_w[k] = tok
            self.readers[k] = []
        for k in reads:
            if k in writes:
                continue
            self.readers.setdefault(k, []).append(tok)
            if len(self.readers[k]) > 64:
                best = {}
                for (kk, vv) in self.readers[k]:
                    if best.get(kk, 0) < vv:
                        best[kk] = vv
                self.readers[k] = list(best.items())

    def op(self, e, fn, reads=(), writes=()):
        deps = self._deps(reads, writes)
        self._wait(e, deps)
        ins = fn(self.eng[e])
        k = "e:" + e
        self.cnt[k] += 1
        ins.then_inc(self.sem[k], 1)
        self._record((k, self.cnt[k]), reads, writes)

    def dma(self, e, out, in_, reads=(), writes=(), sem=None, **kw):
        deps = self._deps(reads, writes)
        self._wait(e, deps)
        k = self.dsem(sem)
        ins = self.eng[e].dma_start(out=out, in_=in_, **kw)
        self.cnt[k] += 16
        ins.then_inc(self.sem[k], 16)
        self._record((k, self.cnt[k]), reads, writes)

    def bank(self):
        b = self.nbank % 8
        self.nbank += 1
        return b

    def slot(self):
        s = self.nslot % NSLOT
        self.nslot += 1
        return s

    def barrier(self):
        allv = {k: v for k, v in self.cnt.items() if v > 0}
        for e in self.eng:
            self._wait(e, dict(allv))
        self.last_w.clear()
        self.readers.clear()

    def finish(self, e="sp"):
        allv = {k: v for k, v in self.cnt.items() if v > 0}
        self._wait(e, allv)


class Prog:
    def __init__(self, cfg):
        self.cfg = cfg
        self.nc = bass.Bass("TRN2", target_bir_lowering=False)
        self.es = ExitStack()
        self.S = None

    def din(self, name, shape, dt=F32):
        return self.nc.dram_tensor(name, list(shape), dt, kind="ExternalInput").ap()

    def dscr(self, name, shape, dt=F32):
        return self.nc.dram_tensor(name, list(shape), dt).ap()

    def sb(self, st, name, shape, dt=F32):
        self._nid = getattr(self, "_nid", 0) + 1
        return st.enter_context(self.nc.sbuf_tensor("%s_%d" % (name, self._nid), list(shape), dt))

    def build(self):
        cfg = self.cfg
        nc = self.nc
        D, T, FF, E, PLE, L, TQ = cfg.D, cfg.T, cfg.FF, cfg.E, cfg.PLE, cfg.L, cfg.TQ
        DC, FC, NQ, NCH = cfg.DC, cfg.FC, cfg.NQ, cfg.NCH
        NDENSE, NMOE = (L + 1) // 2, L // 2
        I = {}
        I["x"] = self.din("x", [T, D])
        I["p"] = self.din("p", [L, T, PLE])
        I["attn_norm"] = self.din("attn_norm", [L, 128, DC])
        I["w_in"] = self.din("w_in", [L, D, cfg.NIN])
        I["conv_dn"] = self.din("conv_dn", [L, 1536, 4])
        I["conv_ml"] = self.din("conv_ml", [L, 1024, 4])
        for nm in ("dn_a_log", "dn_dt_bias", "ml_i_bias", "ml_f_bias"):
            I[nm] = self.din(nm, [L, 4, 1])
        for nm in ("dn_norm", "diff_norm", "ml_norm"):
            I[nm] = self.din(nm, [L, 128, 1])
        for nm in ("diff_lq1", "diff_lk1", "diff_lq2", "diff_lk2"):
            I[nm] = self.din(nm, [L, 64])
        I["w_out"] = self.din("w_out", [L, 2048, D])
        I["ffn_norm"] = self.din("ffn_norm", [L, 128, DC])
        I["dense_w_gate"] = self.din("dense_w_gate", [NDENSE, D, FF])
        I["dense_w_up"] = self.din("dense_w_up", [NDENSE, D, FF])
        I["dense_w_down"] = self.din("dense_w_down", [NDENSE, FF, D])
        I["router"] = self.din("router", [max(NMOE, 1), D, E])
        I["moe_w_gate"] = self.din("moe_w_gate", [max(NMOE, 1), E, D, FF])
        I["moe_w_up"] = self.din("moe_w_up", [max(NMOE, 1), E, D, FF])
        I["moe_w_down"] = self.din("moe_w_down", [max(NMOE, 1), E, FF, D])
        I["ple_norm"] = self.din("ple_norm", [L, 128, DC])
        I["ple_proj"] = self.din("ple_proj", [L, PLE, D])
        I["ple_gate"] = self.din("ple_gate", [L, D, D])
        I["final_norm"] = self.din("final_norm", [128, DC])
        I["c_ident"] = self.din("c_ident", [128, 128])
        I["c_mui"] = self.din("c_mui", [128, 128])
        I["c_mus"] = self.din("c_mus", [128, 128])
        I["c_cmask"] = self.din("c_cmask", [4, T])
        I["c_qaug"] = self.din("c_qaug", [NDH, 3, T])
        I["c_kaug"] = self.din("c_kaug", [NDH, 3, T])
        self.I = I
        self.out = nc.dram_tensor("out", [T, D], F32, kind="ExternalOutput").ap()
        self.hT = self.dscr("hT", [D, T])
        self.zT = self.dscr("zT", [7184, T])
        self.mixT = self.dscr("mixT", [2048, T], BF16)
        self.hnTd = self.dscr("hnTd", [D, T], BF16)
        self.gTd = self.dscr("gTd", [8, T])
        self.actd = self.dscr("actd", [E, FF, T], BF16)

        es = self.es
        S = self.S = Sched(nc, es)
        self.psum = es.enter_context(nc.psum_tensor("ps", [128, 8, 512], F32))
        self.ident = self.sb(es, "ident", [128, 128])
        self.ones32 = self.sb(es, "ones32", [128, 128])
        self.ones16 = self.sb(es, "ones16", [128, 128], BF16)
        self.mui = self.sb(es, "mui", [128, 128])
        self.mus = self.sb(es, "mus", [128, 128])
        self.mui16 = self.sb(es, "mui16", [128, 128], BF16)
        S.dma("sp", self.ident[:], I["c_ident"], writes=["ident"], sem="c_ident")
        S.dma("sp", self.mui[:], I["c_mui"], writes=["mui"], sem="c_mui")
        S.dma("sp", self.mus[:], I["c_mus"], writes=["mus"], sem="c_mus")
        S.op("dve", lambda e: e.memset(self.ones32[:], 1.0), writes=["ones32"])
        S.op("dve", lambda e: e.memset(self.ones16[:], 1.0), writes=["ones16"])
        S.op("dve", lambda e: e.tensor_copy(out=self.mui16[:], in_=self.mui[:]), reads=["mui"], writes=["mui16"])

        stop = ""
        self.token_phase(first=True, layer=0)
        for l in range(L):
            if stop == "tp0":
                break
            S.barrier()
            self.mixer_phase(l)
            if stop == "mix0":
                break
            S.barrier()
            self.token_phase(first=False, layer=l, mode="A")
            self.ffn_up_phase(l)
            self.token_phase(first=False, layer=l, mode="B")
            if stop == "tp1":
                break
        S.finish("sp")
        for e in ("pe", "dve", "act", "pool"):
            S.finish(e)
        self.es.close()
        return nc

    def ps(self, b, n=128, w=512):
        return self.psum[:n, b, :w]

    def evac_engine(self):
        self._ev = getattr(self, "_ev", 0) + 1
        return "act" if self._ev % 2 else "dve"

    def transpose_to(self, src_ap, src_key, np_in, nf_in, bank, col0=0):
        S = self.S
        out = self.psum[:nf_in, bank, col0:col0 + np_in]
        S.op("pe", lambda e: e.transpose(out, src_ap, self.ident[:np_in, :np_in]),
             reads=[src_key, "ident"], writes=["ps%d" % bank])

    def gemm_ksplit(self, K, w, ncols, rhs_fn, epi_fn):
        S, cfg = self.S, self.cfg
        KC = K // 128
        KB = SLOT // 512
        kblocks = [(k0, min(KB, KC - k0)) for k0 in range(0, KC, KB)]
        blocks = []
        for cg0 in range(0, ncols, 512):
            ncg = min(512, ncols - cg0)
            for kbi, (k0, nk) in enumerate(kblocks):
                blocks.append({"cg0": cg0, "ncg": ncg, "kbi": kbi, "k0": k0, "nk": nk})
        issued = [0]

        def issue(bi):
            blk = blocks[bi]
            s_ = S.slot()
            blk["slot"] = s_
            nk, ncg = blk["nk"], blk["ncg"]
            view = self.wslots[:, s_, 0:nk * ncg].rearrange("p (c n) -> p c n", n=ncg)
            blk["view"] = view
            src = w[blk["k0"] * 128:(blk["k0"] + nk) * 128, blk["cg0"]:blk["cg0"] + ncg].rearrange("(c p) n -> p c n", p=128)
            half = nk // 2 if nk >= 8 else nk
            first = True
            for a in range(0, nk, half):
                S.dma("pool", view[:, a:a + half, :], src[:, a:a + half, :], writes=["w%d" % s_] if first else [], sem="w%d" % s_)
                first = False
            k = S.dsem("w%d" % s_)
            S.last_w["w%d" % s_] = (k, S.cnt[k])

        banks = None
        for bi, blk in enumerate(blocks):
            while issued[0] < min(len(blocks), bi + NSLOT - 1):
                issue(issued[0])
                issued[0] += 1
            nch = blk["ncg"] // 128
            if blk["kbi"] == 0:
                banks = [S.bank() for _ in range(nch)]
            lastkb = blk["kbi"] == len(kblocks) - 1
            for ci in range(nch):
                b = banks[ci]
                rk = list(set(rhs_fn(blk["k0"] + kk)[1] for kk in range(blk["nk"])))

                def mm(e, blk=blk, ci=ci, b=b, lastkb=lastkb):
                    ins = None
                    for kk in range(blk["nk"]):
                        ins = e.matmul(self.psum[:, b, :cfg.TQ], lhsT=blk["view"][:, kk, ci * 128:(ci + 1) * 128],
                                       rhs=rhs_fn(blk["k0"] + kk)[0], start=(blk["kbi"] == 0 and kk == 0),
                                       stop=(lastkb and kk == blk["nk"] - 1))
                    return ins
                S.op("pe", mm, reads=["w%d" % blk["slot"]] + rk, writes=["ps%d" % b])
            if lastkb:
                for ci in range(nch):
                    epi_fn(blk["cg0"] // 128 + ci, banks[ci], 128)

    def gemm(self, K, chunks, rhs_fn, epi_fn, nparts=1):
        S, cfg = self.S, self.cfg
        KC = K // 128
        maxcols = SLOT // KC
        blocks = []
        for idx, (w, c0, n) in enumerate(chunks):
            if blocks and blocks[-1]["w"] is w and blocks[-1]["c0"] + blocks[-1]["n"] == c0 \
                    and blocks[-1]["n"] + n <= maxcols:
                blocks[-1]["ch"].append((idx, blocks[-1]["n"], n))
                blocks[-1]["n"] += n
            else:
                blocks.append({"w": w, "c0": c0, "n": n, "ch": [(idx, 0, n)]})
        issued = [0]

        def issue(bi):
            blk = blocks[bi]
            s = S.slot()
            blk["slot"] = s
            view = self.wslots[:, s, 0:KC * blk["n"]].rearrange("p (c n) -> p c n", n=blk["n"])
            blk["view"] = view
            src = blk["w"][:, blk["c0"]:blk["c0"] + blk["n"]].rearrange("(c p) n -> p c n", p=128)
            half = KC // 2 if KC >= 8 else KC
            for a in range(0, KC, half):
                S.dma("pool", view[:, a:a + half, :], src[:, a:a + half, :], writes=["w%d" % s] if a == 0 else [],
                      reads=[] if a == 0 else [], sem="w%d" % s)
            k = S.dsem("w%d" % s)
            S.last_w["w%d" % s] = (k, S.cnt[k])

        for bi, blk in enumerate(blocks):
            while issued[0] < min(len(blocks), bi + NSLOT - 1):
                issue(issued[0])
                issued[0] += 1
            for (idx, off, n) in blk["ch"]:
              for part in range(nparts):
                b = S.bank()
                rf = (lambda kc: rhs_fn(kc)) if nparts == 1 else (lambda kc, part=part: rhs_fn(kc, part))
                rk = [rf(kc)[1] for kc in range(KC)]

                def mm(e, blk=blk, off=off, n=n, b=b, rf=rf):
                    ins = None
                    for kc in range(KC):
                        ins = e.matmul(self.psum[:n, b, :cfg.TQ], lhsT=blk["view"][:, kc, off:off + n],
                                       rhs=rf(kc)[0], start=(kc == 0), stop=(kc == KC - 1))
                    return ins
                S.op("pe", mm, reads=["w%d" % blk["slot"]] + list(set(rk)), writes=["ps%d" % b])
                if nparts == 1:
                    epi_fn(idx, b, n)
                else:
                    epi_fn(idx, b, n, part)

    def norm(self, wname, layer, out_fn=None):
        S, cfg = self.S, self.cfg
        DC, TQ, D = cfg.DC, cfg.TQ, cfg.D
        wv = self.I[wname] if layer is None else self.I[wname][layer]
        S.dma("sp", self.nw[:], wv, writes=["nw"], sem="nw")
        b = S.bank()
        for kc in range(DC):
            sl = kc % 2
            S.op("act", lambda e, kc=kc, sl=sl: e.activation(out=self.sq[:, sl, :], in_=self.h_q[:, kc, :], func=AF.Square),
                 reads=["h%d" % kc], writes=["sq%d" % sl])
            S.op("pe", lambda e, kc=kc, sl=sl: e.matmul(self.psum[:, b, :TQ], lhsT=self.ones16[:], rhs=self.sq[:, sl, :],
                                                        start=(kc == 0), stop=(kc == DC - 1)),
                 reads=["sq%d" % sl, "ones16"], writes=["ps%d" % b] if kc == 0 else [])
        S.last_w["ps%d" % b] = ("e:pe", S.cnt["e:pe"])
        S.op("act", lambda e: e.activation(out=self.rstd[:], in_=self.psum[:, b, :TQ], func=AF.Sqrt, scale=1.0 / D, bias=self.epsb[:]),
             reads=["ps%d" % b, "epsb"], writes=["rstd"])
        S.op("dve", lambda e: e.reciprocal(out=self.rstd[:], in_=self.rstd[:]), reads=["rstd"], writes=["rstd"])
        for kc in range(DC):
            if out_fn is None:
                S.op("dve", lambda e, kc=kc: e.scalar_tensor_tensor(out=self.hn_q[:, kc, :], in0=self.h_q[:, kc, :],
                                                                    scalar=self.nw[:, kc:kc + 1], in1=self.rstd[:],
                                                                    op0=ALU.mult, op1=ALU.mult),
                     reads=["h%d" % kc, "nw", "rstd"], writes=["hn%d" % kc])
            else:
                out_fn(kc)

    def token_phase(self, first, layer, mode=""):
        S, cfg, I, nc = self.S, self.cfg, self.I, self.nc
        D, T, FF, E, PLE, L, TQ = cfg.D, cfg.T, cfg.FF, cfg.E, cfg.PLE, cfg.L, cfg.TQ
        DC, FC, NQ = cfg.DC, cfg.FC, cfg.NQ
        with ExitStack() as st:
            self.wslots = self.sb(st, "wslots", [128, NSLOT, SLOT], BF16)
            self.h_q = self.sb(st, "h_q", [128, DC, TQ])
            self.hn_q = self.sb(st, "hn_q", [128, DC, TQ], BF16)
            nbig = max(FC, 2 * DC, 16)
            self.big = self.sb(st, "big", [128, nbig, TQ], BF16)
            self.sq = self.sb(st, "sq", [128, 2, TQ], BF16)
            self.rstd = self.sb(st, "rstd", [128, TQ])
            self.nw = self.sb(st, "nw", [128, DC])
            self.epsb = self.sb(st, "epsb", [128, 1])
            self.stage = self.sb(st, "stage", [128, 4, TQ])
            self.tmp = self.sb(st, "tmp", [128, 2, TQ])
            self.xin = self.sb(st, "xin", [128, 2, 128])
            self.gbc = self.sb(st, "gbc", [128, 2, TQ])
            self.rt32 = self.sb(st, "rt32", [128, DC, E])
            self.lgT = self.sb(st, "lgT", [8, TQ])
            self.gT = self.sb(st, "gT", [8, TQ])
            self.lgw = self.sb(st, "lgw", [128, 8, 32])
            self.sel = self.sb(st, "sel", [8, E, 128])
            self.pT = self.sb(st, "pT", [128, max(PLE // 128, 1), TQ], BF16)
            pp32 = self.big[:].rearrange("p c t -> p (c t)").bitcast(F32)[:, 0:DC * TQ].rearrange("p (c t) -> p c t", t=TQ)
            S.op("dve", lambda e: e.memset(self.epsb[:], EPS), writes=["epsb"])
            for ee in range(E):
                S.op("dve", lambda e, ee=ee: e.tensor_scalar(out=self.sel[:, ee, :], in0=self.ones32[:8, :],
                                                            scalar1=self.ident[:8, ee:ee + 1], scalar2=None, op0=ALU.mult),
                     reads=["ones32", "ident"], writes=["sel"])
            self._stg = 0

            def hkeys():
                return ["h%d" % k for k in range(DC)]

            def hnrhs(kc):
                return (self.hn_q[:, kc, :], "hn%d" % kc)

            for q in range(NQ):
                t0 = q * TQ
                if first:
                    for tt in range(TQ // 128):
                        for dc in range(DC):
                            sl = (tt * DC + dc) % 2
                            S.dma("sp", self.xin[:, sl, :], I["x"][t0 + tt * 128:t0 + (tt + 1) * 128, dc * 128:(dc + 1) * 128],
                                  writes=["xin%d" % sl], sem="xin%d" % sl)
                            b = S.bank()
                            self.transpose_to(self.xin[:, sl, :], "xin%d" % sl, 128, 128, b)
                            S.op(self.evac_engine(), lambda e, dc=dc, tt=tt, b=b: (e.tensor_copy(out=self.h_q[:, dc, tt * 128:(tt + 1) * 128], in_=self.psum[:, b, :128])
                                                                                     if e is nc.vector else e.copy(out=self.h_q[:, dc, tt * 128:(tt + 1) * 128], in_=self.psum[:, b, :128])),
                                 reads=["ps%d" % b], writes=["h%d" % dc])
                elif mode == "A":
                    S.dma("sp", self.h_q[:], self.hT[:, t0:t0 + TQ].rearrange("(c p) t -> p c t", p=128), writes=hkeys(), sem="hq")
                    l = layer
                    mix = self.big[:, 0:16, :]
                    S.dma("sp", mix, self.mixT[:, t0:t0 + TQ].rearrange("(c p) t -> p c t", p=128), writes=["big"], sem="big")

                    def epi_add(idx, b, n):
                        S.op("dve", lambda e: e.tensor_tensor(out=self.h_q[:, idx, :], in0=self.psum[:, b, :TQ], in1=self.h_q[:, idx, :], op=ALU.add),
                             reads=["ps%d" % b, "h%d" % idx], writes=["h%d" % idx])
                    self.gemm(2048, [(I["w_out"][l], c * 128, 128) for c in range(DC)], lambda kc: (self.big[:, kc, :], "big"), epi_add)
                    self.norm("ffn_norm", l)
                    j = l // 2
                    if l % 2 == 1:
                        self.router(j)
                        S.dma("sp", self.gTd[:, t0:t0 + TQ], self.gT[:], reads=["gT"], sem="gTst")
                    S.dma("sp", self.hnTd[:, t0:t0 + TQ].rearrange("(c p) t -> p c t", p=128), self.hn_q[:],
                          reads=["hn%d" % k for k in range(DC)], sem="hnst")
                    S.dma("sp", self.hT[:, t0:t0 + TQ].rearrange("(c p) t -> p c t", p=128), self.h_q[:], reads=hkeys(), sem="hq")
                    continue
                if mode == "B":
                    l = layer
                    j = l // 2
                    S.dma("sp", self.h_q[:], self.hT[:, t0:t0 + TQ].rearrange("(c p) t -> p c t", p=128), writes=hkeys(), sem="hq")
                    if l % 2 == 1:
                        S.dma("sp", self.gT[:], self.gTd[:, t0:t0 + TQ], writes=["gT"], sem="gTst")
                    for ee in range(E if l % 2 == 1 else 1):
                        wd = I["moe_w_down"][j, ee] if l % 2 == 1 else I["dense_w_down"][j]
                        S.dma("sp", self.big[:, 0:FC, :], self.actd[ee, :, t0:t0 + TQ].rearrange("(c p) t -> p c t", p=128),
                              writes=["big"], sem="big")
                        self.ffn_down(wd, ee if l % 2 == 1 else None)
                    self.norm("ple_norm", l)
                    for tt in range(TQ // 128):
                        for pc in range(PLE // 128):
                            sl = (tt * 2 + pc) % 2
                            S.dma("sp", self.xin[:, sl, :], I["p"][l, t0 + tt * 128:t0 + (tt + 1) * 128, pc * 128:(pc + 1) * 128],
                                  writes=["xin%d" % sl], sem="xin%d" % sl)
                            b = S.bank()
                            self.transpose_to(self.xin[:, sl, :], "xin%d" % sl, 128, 128, b)
                            S.op("dve", lambda e, pc=pc, tt=tt, b=b: e.tensor_copy(out=self.pT[:, pc, tt * 128:(tt + 1) * 128], in_=self.psum[:, b, :128]),
                                 reads=["ps%d" % b], writes=["pT"])

                    def epi_pp(idx, b, n):
                        S.op("act", lambda e: e.copy(out=pp32[:, idx, :], in_=self.psum[:, b, :TQ]), reads=["ps%d" % b], writes=["big"])
                    self.gemm(PLE, [(I["ple_proj"][l], c * 128, 128) for c in range(DC)], lambda kc: (self.pT[:, kc, :], "pT"), epi_pp)

                    def epi_gate(idx, b, n):
                        sl = idx % 2
                        S.op("act", lambda e: e.activation(out=self.tmp[:, sl, :], in_=self.psum[:, b, :TQ], func=AF.Sigmoid),
                             reads=["ps%d" % b], writes=["tmp%d" % sl])
                        S.op("dve", lambda e: e.tensor_tensor(out=self.tmp[:, sl, :], in0=self.tmp[:, sl, :], in1=pp32[:, idx, :], op=ALU.mult),
                             reads=["tmp%d" % sl, "big"], writes=["tmp%d" % sl])
                        S.op("dve", lambda e: e.tensor_tensor(out=self.h_q[:, idx, :], in0=self.tmp[:, sl, :], in1=self.h_q[:, idx, :], op=ALU.add),
                             reads=["tmp%d" % sl, "h%d" % idx], writes=["h%d" % idx])
                    self.gemm(D, [(I["ple_gate"][l], c * 128, 128) for c in range(DC)], hnrhs, epi_gate)

                nxt = 0 if first else layer + 1
                if nxt < L:
                    S.dma("sp", self.hT[:, t0:t0 + TQ].rearrange("(c p) t -> p c t", p=128), self.h_q[:], reads=hkeys(), sem="hq")
                    self.norm("attn_norm", nxt)
                    w = I["w_in"][nxt]
                    chunks = [(w, c * 128, 128) for c in range(24)] + [(w, 3080 + c * 128, 128) for c in range(32)] \
                        + [(w, 3072, 8), (w, 7176, 8)]

                    def epi_z(idx, b, n, t0=t0):
                        sl = self._stg % 4
                        self._stg += 1
                        eng = self.evac_engine()
                        S.op(eng, lambda e: (e.tensor_copy(out=self.stage[:n, sl, :], in_=self.psum[:n, b, :TQ]) if e is nc.vector
                                             else e.copy(out=self.stage[:n, sl, :], in_=self.psum[:n, b, :TQ])),
                             reads=["ps%d" % b], writes=["stg%d" % sl])
                        r0 = idx * 128 if idx < 56 else (7168 if idx == 56 else 7176)
                        S.dma("sp", self.zT[r0:r0 + n, t0:t0 + TQ], self.stage[:n, sl, :], reads=["stg%d" % sl], sem="stg%d" % sl)
                    self.gemm(D, chunks, hnrhs, epi_z)
                else:
                    def fin(kc):
                        S.op("dve", lambda e: e.scalar_tensor_tensor(out=self.h_q[:, kc, :], in0=self.h_q[:, kc, :], scalar=self.nw[:, kc:kc + 1],
                                                                     in1=self.rstd[:], op0=ALU.mult, op1=ALU.mult),
                             reads=["h%d" % kc, "nw", "rstd"], writes=["h%d" % kc])
                    self.norm("final_norm", None, out_fn=fin)
                    for tt in range(TQ // 128):
                        for dg in range(0, DC, 4):
                            b = S.bank()
                            ng = min(4, DC - dg)
                            for i in range(ng):
                                self.transpose_to(self.h_q[:, dg + i, tt * 128:(tt + 1) * 128], "h%d" % (dg + i), 128, 128, b, col0=i * 128)
                            sl = self._stg % 4
                            self._stg += 1
                            S.op(self.evac_engine(), lambda e, b=b, sl=sl, ng=ng: (e.tensor_copy(out=self.stage[:, sl, :ng * 128], in_=self.psum[:, b, :ng * 128]) if e is nc.vector
                                                                                 else e.copy(out=self.stage[:, sl, :ng * 128], in_=self.psum[:, b, :ng * 128])),
                                 reads=["ps%d" % b], writes=["stg%d" % sl])
                            S.dma("sp", self.out[t0 + tt * 128:t0 + (tt + 1) * 128, dg * 128:(dg + ng) * 128], self.stage[:, sl, :ng * 128],
                                  reads=["stg%d" % sl], sem="stg%d" % sl)
            S.barrier()

    def ffn(self, wg, wu, wd, expert):
        S, cfg = self.S, self.cfg
        TQ, DC, FC, D, FF = cfg.TQ, cfg.DC, cfg.FC, cfg.D, cfg.FF
        chunks = []
        for c in range(0, FC, 2):
            n2 = min(2, FC - c)
            chunks += [(wg, (c + i) * 128, 128) for i in range(n2)] + [(wu, (c + i) * 128, 128) for i in range(n2)]
        order = []
        for c in range(0, FC, 2):
            n2 = min(2, FC - c)
            order += [("g", c + i) for i in range(n2)] + [("u", c + i) for i in range(n2)]
        gbank = {}

        def epi(idx, b, n):
            kind, fc = order[idx]
            if kind == "g":
                sl = fc % 2
                S.op("act", lambda e: e.activation(out=self.tmp[:, sl, :], in_=self.psum[:, b, :TQ], func=AF.Silu),
                     reads=["ps%d" % b], writes=["tmp%d" % sl])
                gbank[fc] = sl
            else:
                sl = gbank.pop(fc)
                S.op("dve", lambda e: e.tensor_tensor(out=self.big[:, fc, :], in0=self.psum[:, b, :TQ], in1=self.tmp[:, sl, :], op=ALU.mult),
                     reads=["ps%d" % b, "tmp%d" % sl], writes=["big"])
        self.gemm(D, chunks, lambda kc: (self.hn_q[:, kc, :], "hn%d" % kc), epi)

        def epi_d(idx, b, n):
            if expert is None:
                S.op("dve", lambda e: e.tensor_tensor(out=self.h_q[:, idx, :], in0=self.psum[:, b, :TQ], in1=self.h_q[:, idx, :], op=ALU.add),
                     reads=["ps%d" % b, "h%d" % idx], writes=["h%d" % idx])
            else:
                sl = idx % 2
                S.op("dve", lambda e: e.tensor_tensor(out=self.tmp[:, sl, :], in0=self.psum[:, b, :TQ], in1=self.gbc[:, expert % 2, :], op=ALU.mult),
                     reads=["ps%d" % b, "gbc%d" % (expert % 2)], writes=["tmp%d" % sl])
                S.op("pool", lambda e: e.tensor_tensor(out=self.h_q[:, idx, :], in0=self.tmp[:, sl, :], in1=self.h_q[:, idx, :], op=ALU.add),
                     reads=["tmp%d" % sl, "h%d" % idx], writes=["h%d" % idx])
        if expert is not None:
            b = S.bank()
            S.op("pe", lambda e: e.matmul(self.psum[:, b, :TQ], lhsT=self.sel[:, expert, :], rhs=self.gT[:], start=True, stop=True),
                 reads=["sel", "gT"], writes=["ps%d" % b])
            S.op("act", lambda e: e.copy(out=self.gbc[:, expert % 2, :], in_=self.psum[:, b, :TQ]), reads=["ps%d" % b], writes=["gbc%d" % (expert % 2)])
        self.gemm(FF, [(wd, c * 128, 128) for c in range(DC)], lambda kc: (self.big[:, kc, :], "big"), epi_d)

    def ffn_down(self, wd, expert):
        S, cfg = self.S, self.cfg
        TQ, DC, FC, D, FF = cfg.TQ, cfg.DC, cfg.FC, cfg.D, cfg.FF

        def epi_d(idx, b, n):
            if expert is None:
                S.op("dve", lambda e: e.tensor_tensor(out=self.h_q[:, idx, :], in0=self.psum[:, b, :TQ], in1=self.h_q[:, idx, :], op=ALU.add),
                     reads=["ps%d" % b, "h%d" % idx], writes=["h%d" % idx])
            else:
                sl = idx % 2
                S.op("dve", lambda e: e.tensor_tensor(out=self.tmp[:, sl, :], in0=self.psum[:, b, :TQ], in1=self.gbc[:, expert % 2, :], op=ALU.mult),
                     reads=["ps%d" % b, "gbc%d" % (expert % 2)], writes=["tmp%d" % sl])
                S.op("pool", lambda e: e.tensor_tensor(out=self.h_q[:, idx, :], in0=self.tmp[:, sl, :], in1=self.h_q[:, idx, :], op=ALU.add),
                     reads=["tmp%d" % sl, "h%d" % idx], writes=["h%d" % idx])
        if expert is not None:
            b = S.bank()
            S.op("pe", lambda e: e.matmul(self.psum[:, b, :TQ], lhsT=self.sel[:, expert, :], rhs=self.gT[:], start=True, stop=True),
                 reads=["sel", "gT"], writes=["ps%d" % b])
            S.op("act", lambda e: e.copy(out=self.gbc[:, expert % 2, :], in_=self.psum[:, b, :TQ]), reads=["ps%d" % b], writes=["gbc%d" % (expert % 2)])
        self.gemm_ksplit(FF, wd, D, lambda kc: (self.big[:, kc, :], "big"), epi_d)

    def ffn_up_phase(self, l):
        S, cfg, I = self.S, self.cfg, self.I
        TQ, DC, FC, D, FF, T, E = cfg.TQ, cfg.DC, cfg.FC, cfg.D, cfg.FF, cfg.T, cfg.E
        NP = T // TQ
        j = l // 2
        with ExitStack() as st:
            self.wslots = self.sb(st, "wslots", [128, NSLOT, SLOT], BF16)
            hn_all = self.sb(st, "hn_all", [128, DC, T], BF16)
            sgbuf = self.sb(st, "sgbuf", [128, 4, T], BF16)
            ast = self.sb(st, "ast", [128, 4, TQ], BF16)
            S.dma("sp", hn_all[:], self.hnTd.rearrange("(c p) t -> p c t", p=128), writes=["hn_all"], sem="hn_all")
            nst = [0]
            for ee in range(E if l % 2 == 1 else 1):
                wg = I["moe_w_gate"][j, ee] if l % 2 == 1 else I["dense_w_gate"][j]
                wu = I["moe_w_up"][j, ee] if l % 2 == 1 else I["dense_w_up"][j]
                chunks, order = [], []
                for c0 in range(0, FC, 4):
                    n4 = min(4, FC - c0)
                    chunks += [(wg, (c0 + i) * 128, 128) for i in range(n4)] + [(wu, (c0 + i) * 128, 128) for i in range(n4)]
                    order += [("g", c0 + i) for i in range(n4)] + [("u", c0 + i) for i in range(n4)]

                def epi(idx, b, n, part, ee=ee, order=order):
                    kind, fc = order[idx]
                    key = "sg%d_%d" % (fc % 4, part)
                    if kind == "g":
                        S.op("act", lambda e: e.activation(out=sgbuf[:, fc % 4, part * TQ:(part + 1) * TQ], in_=self.psum[:, b, :TQ], func=AF.Silu),
                             reads=["ps%d" % b], writes=[key])
                    else:
                        sl = nst[0] % 4
                        nst[0] += 1
                        S.op("dve", lambda e: e.tensor_tensor(out=ast[:, sl, :], in0=self.psum[:, b, :TQ], in1=sgbuf[:, fc % 4, part * TQ:(part + 1) * TQ], op=ALU.mult),
                             reads=["ps%d" % b, key], writes=["ast%d" % sl])
                        S.dma("sp", self.actd[ee, fc * 128:(fc + 1) * 128, part * TQ:(part + 1) * TQ], ast[:, sl, :], reads=["ast%d" % sl], sem="ast%d" % sl)
                self.gemm(D, chunks, lambda kc, part: (hn_all[:, kc, part * TQ:(part + 1) * TQ], "hn_all"), epi, nparts=NP)
            S.barrier()

    def router(self, j):
        S, cfg, I = self.S, self.cfg, self.I
        TQ, DC, E = cfg.TQ, cfg.DC, cfg.E
        S.dma("sp", self.rt32[:], I["router"][j].rearrange("(c p) e -> p c e", p=128), writes=["rt32"], sem="rt32")
        b = S.bank()
        for kc in range(DC):
            sl = kc % 2
            S.op("dve", lambda e, kc=kc, sl=sl: e.scalar_tensor_tensor(out=self.tmp[:, sl, :], in0=self.h_q[:, kc, :], scalar=self.nw[:, kc:kc + 1],
                                                                      in1=self.rstd[:], op0=ALU.mult, op1=ALU.mult),
                 reads=["h%d" % kc, "nw", "rstd"], writes=["tmp%d" % sl])
            S.op("pe", lambda e, kc=kc, sl=sl: e.matmul(self.psum[:E, b, :TQ], lhsT=self.rt32[:, kc, :], rhs=self.tmp[:, sl, :],
                                                        start=(kc == 0), stop=(kc == DC - 1)),
                 reads=["rt32", "tmp%d" % sl], writes=["ps%d" % b] if kc == 0 else [])
        S.last_w["ps%d" % b] = ("e:pe", S.cnt["e:pe"])
        S.op("act", lambda e: e.copy(out=self.lgT[:], in_=self.psum[:E, b, :TQ]), reads=["ps%d" % b], writes=["lgT"])
        NT = TQ // 128
        b2 = S.bank()
        for tt in range(NT):
            self.transpose_to(self.lgT[:, tt * 128:(tt + 1) * 128], "lgT", E, 128, b2, col0=tt * 8)
        W = self.lgw
        S.op("dve", lambda e: e.tensor_copy(out=W[:, 0, :NT * 8], in_=self.psum[:, b2, :NT * 8]), reads=["ps%d" % b2], writes=["lgw"])
        v = lambda k: W[:, k, :NT * 8].rearrange("p (t e) -> p t e", e=8)
        v1 = lambda k: W[:, k, :NT]
        S.op("dve", lambda e: e.tensor_reduce(out=v1(1), in_=v(0), axis=mybir.AxisListType.X, op=ALU.max), reads=["lgw"], writes=["lgw"])
        for tt in range(NT):
            lt = W[:, 0, tt * 8:(tt + 1) * 8]
            m1 = W[:, 1, tt:tt + 1]
            S.op("dve", lambda e, lt=lt, m1=m1, tt=tt: e.tensor_scalar(out=W[:, 2, tt * 8:(tt + 1) * 8], in0=lt, scalar1=m1, scalar2=-1e30,
                                                                      op0=ALU.is_equal, op1=ALU.mult), reads=["lgw"], writes=["lgw"])
            S.op("dve", lambda e, lt=lt, tt=tt: e.tensor_tensor(out=W[:, 2, tt * 8:(tt + 1) * 8], in0=W[:, 2, tt * 8:(tt + 1) * 8], in1=lt, op=ALU.add),
                 reads=["lgw"], writes=["lgw"])
            S.op("dve", lambda e, tt=tt: e.tensor_reduce(out=W[:, 3, tt:tt + 1], in_=W[:, 2, tt * 8:(tt + 1) * 8], axis=mybir.AxisListType.X, op=ALU.max),
                 reads=["lgw"], writes=["lgw"])
            m2 = W[:, 3, tt:tt + 1]
            S.op("dve", lambda e, lt=lt, m2=m2, tt=tt: e.tensor_scalar(out=W[:, 4, tt * 8:(tt + 1) * 8], in0=lt, scalar1=m2, scalar2=None, op0=ALU.is_ge),
                 reads=["lgw"], writes=["lgw"])
            S.op("dve", lambda e, m1=m1, tt=tt: e.tensor_scalar(out=W[:, 5, tt:tt + 1], in0=m1, scalar1=-1.0, scalar2=None, op0=ALU.mult),
                 reads=["lgw"], writes=["lgw"])
            S.op("act", lambda e, lt=lt, tt=tt: e.activation(out=W[:, 6, tt * 8:(tt + 1) * 8], in_=lt, func=AF.Exp, bias=W[:, 5, tt:tt + 1], scale=1.0),
                 reads=["lgw"], writes=["lgw"])
            S.op("act", lambda e, m2=m2, tt=tt: e.activation(out=W[:, 7, tt:tt + 1], in_=m2, func=AF.Exp, bias=W[:, 5, tt:tt + 1], scale=1.0),
                 reads=["lgw"], writes=["lgw"])
            S.op("dve", lambda e, tt=tt: e.tensor_scalar(out=W[:, 7, tt:tt + 1], in0=W[:, 7, tt:tt + 1], scalar1=1.0, scalar2=None, op0=ALU.add),
                 reads=["lgw"], writes=["lgw"])
            S.op("dve", lambda e, tt=tt: e.reciprocal(out=W[:, 7, tt:tt + 1], in_=W[:, 7, tt:tt + 1]), reads=["lgw"], writes=["lgw"])
            S.op("dve", lambda e, tt=tt: e.scalar_tensor_tensor(out=W[:, 4, tt * 8:(tt + 1) * 8], in0=W[:, 6, tt * 8:(tt + 1) * 8], scalar=W[:, 7, tt:tt + 1],
                                                                in1=W[:, 4, tt * 8:(tt + 1) * 8], op0=ALU.mult, op1=ALU.mult),
                 reads=["lgw"], writes=["lgw"])
        b3 = S.bank()
        for tt in range(NT):
            self.transpose_to(W[:, 4, tt * 8:(tt + 1) * 8], "lgw", 128, 8, b3, col0=tt * 128)
        S.op("act", lambda e: e.copy(out=self.gT[:], in_=self.psum[:8, b3, :TQ]), reads=["ps%d" % b3], writes=["gT"])

    def mixer_phase(self, l):
        mixer_phase(self, l)

AX = mybir.AxisListType

def mixer_phase(P, l):
    S, cfg, I, nc = P.S, P.cfg, P.I, P.nc
    T, NCH = cfg.T, cfg.NCH
    lambda_init = 0.8 - 0.6 * math.exp(-0.3 * l)
    ps = P.psum
    ident = P.ident

    def evac(dst, src, rk, wk, eng=None):
        eng = eng or P.evac_engine()
        S.op(eng, lambda e: (e.tensor_copy(out=dst, in_=src) if e is nc.vector else e.copy(out=dst, in_=src)), reads=rk, writes=wk)

    with ExitStack() as st:
        sb = lambda name, shape, dt=F32: P.sb(st, name, shape, dt)
        stg = ExitStack()
        sbg = lambda name, shape, dt=F32: P.sb(stg, name, shape, dt)
        gc = sb("gc", [4, T])
        prm = sb("prm", [4, 8])
        sm = sb("sm", [4, 8, NCH])
        sel4 = sb("sel4", [4, 4, 128])
        ngc = sb("ngc", [4, 4, T])
        ones4 = sb("ones4", [4, 128])
        tok = sb("tok", [128, 6, NCH * 4])
        bc = sb("bc", [128, 2, 4, NCH])
        Gb, Ga, Gi, Gf = sbg("Gb", [4, T]), sbg("Ga", [4, T]), sbg("Gi", [4, T]), sbg("Gf", [4, T])
        cm = sbg("cm", [4, T])
        g1, g2, g3 = sbg("g1", [4, T]), sbg("g2", [4, T]), sbg("g3", [4, T])
        beta, eg, egl = sbg("beta", [4, T]), sbg("eg", [4, T]), sbg("egl", [4, T])
        bb, dd, ww, hden = sbg("bb", [4, T]), sbg("dd", [4, T]), sbg("ww", [4, T]), sbg("hden", [4, T])
        S.dma("sp", Gb[:], P.zT[7168:7172, :], writes=["Gb"], sem="g")
        S.dma("sp", Ga[:], P.zT[7172:7176, :], writes=["Ga"], sem="g")
        S.dma("sp", Gi[:], P.zT[7176:7180, :], writes=["Gi"], sem="g")
        S.dma("sp", Gf[:], P.zT[7180:7184, :], writes=["Gf"], sem="g")
        S.dma("sp", cm[:], I["c_cmask"], writes=["cm"], sem="g")
        for i, nm in enumerate(("dn_a_log", "dn_dt_bias", "ml_i_bias", "ml_f_bias")):
            S.dma("sp", prm[:, i:i + 1], I[nm][l], writes=["prm"], sem="g")
        for k in ("Gb", "Ga", "Gi", "Gf"):
            S.last_w[k] = S.last_w["prm"]
        S.last_w["cm"] = S.last_w["prm"]
        G = ["Gb", "Ga", "Gi", "Gf", "cm", "prm", "g1", "g2", "g3", "beta", "gc", "eg", "egl", "bb", "dd", "ww", "hden", "sm"]

        def gop(eng, fn):
            S.op(eng, fn, reads=G, writes=G)
        gop("dve", lambda e: e.memset(ones4[:], 1.0))
        for h in range(4):
            gop("dve", lambda e, h=h: e.tensor_scalar(out=sel4[:, h, :], in0=ones4[:], scalar1=ident[:4, h:h + 1], scalar2=None, op0=ALU.mult))
        S.last_w["sel4"] = S.last_w["Gb"]
        gop("act", lambda e: e.activation(out=beta[:], in_=Gb[:], func=AF.Sigmoid))
        gop("act", lambda e: e.activation(out=g1[:], in_=Ga[:], func=AF.Exp, bias=prm[:, 1:2], scale=1.0))
        gop("dve", lambda e: e.tensor_scalar(out=g1[:], in0=g1[:], scalar1=1.0, scalar2=None, op0=ALU.add))
        gop("act", lambda e: e.activation(out=g1[:], in_=g1[:], func=AF.Ln))
        gop("act", lambda e: e.activation(out=prm[:, 4:5], in_=prm[:, 0:1], func=AF.Exp))
        gop("dve", lambda e: e.tensor_scalar(out=g1[:], in0=g1[:], scalar1=prm[:, 4:5], scalar2=-1.0, op0=ALU.mult, op1=ALU.mult))
        gop("dve", lambda e: e.tensor_tensor_scan(out=gc[:], data0=cm[:], data1=g1[:], initial=0.0, op0=ALU.mult, op1=ALU.add))
        gop("act", lambda e: e.activation(out=eg[:], in_=gc[:], func=AF.Exp))
        for c in range(NCH):
            last = gc[:, c * 128 + 127:c * 128 + 128]
            gop("dve", lambda e, c=c, last=last: e.tensor_scalar(out=egl[:, c * 128:(c + 1) * 128], in0=gc[:, c * 128:(c + 1) * 128],
                                                                scalar1=-1.0, scalar2=last, op0=ALU.mult, op1=ALU.add))
            gop("act", lambda e, c=c, last=last: e.activation(out=sm[:, 6, c:c + 1], in_=last, func=AF.Exp))
        gop("act", lambda e: e.activation(out=egl[:], in_=egl[:], func=AF.Exp))
        gop("dve", lambda e: e.tensor_scalar(out=g2[:], in0=Gi[:], scalar1=prm[:, 2:3], scalar2=None, op0=ALU.add))
        gop("dve", lambda e: e.tensor_scalar(out=prm[:, 5:6], in0=prm[:, 3:4], scalar1=-1.0, scalar2=None, op0=ALU.mult))
        gop("act", lambda e: e.activation(out=g3[:], in_=Gf[:], func=AF.Exp, bias=prm[:, 5:6], scale=-1.0))
        gop("dve", lambda e: e.tensor_scalar(out=g3[:], in0=g3[:], scalar1=1.0, scalar2=None, op0=ALU.add))
        gop("act", lambda e: e.activation(out=g3[:], in_=g3[:], func=AF.Ln))
        gop("dve", lambda e: e.tensor_scalar(out=g3[:], in0=g3[:], scalar1=-1.0, scalar2=None, op0=ALU.mult))
        gop("dve", lambda e: e.tensor_tensor_scan(out=bb[:], data0=cm[:], data1=g3[:], initial=0.0, op0=ALU.mult, op1=ALU.add))
        gop("dve", lambda e: e.tensor_tensor(out=dd[:], in0=g2[:], in1=bb[:], op=ALU.subtract))
        gop("dve", lambda e: e.tensor_reduce(out=sm[:, 0, :], in_=dd[:].rearrange("p (c t) -> p c t", t=128), axis=AX.X, op=ALU.max))
        gop("dve", lambda e: e.tensor_copy(out=sm[:, 1, :], in_=bb[:].rearrange("p (c t) -> p c t", t=128)[:, :, 127]))
        gop("dve", lambda e: e.tensor_tensor_scan(out=sm[:, 2, :], data0=sm[:, 0, :], data1=sm[:, 1, :], initial=0.0, op0=ALU.max, op1=ALU.add))
        gop("dve", lambda e: e.tensor_tensor(out=sm[:, 3, :], in0=sm[:, 2, :], in1=sm[:, 1, :], op=ALU.subtract))
        gop("dve", lambda e: e.memset(sm[:, 4, :], 0.0))
        if NCH > 1:
            gop("dve", lambda e: e.tensor_copy(out=sm[:, 4, 1:NCH], in_=sm[:, 2, 0:NCH - 1]))
        gop("dve", lambda e: e.tensor_tensor(out=sm[:, 5, :], in0=sm[:, 4, :], in1=sm[:, 3, :], op=ALU.subtract))
        gop("act", lambda e: e.activation(out=sm[:, 5, :], in_=sm[:, 5, :], func=AF.Exp))
        for c in range(NCH):
            Mc = sm[:, 3, c:c + 1]
            gop("dve", lambda e, c=c, Mc=Mc: e.tensor_scalar(out=ww[:, c * 128:(c + 1) * 128], in0=dd[:, c * 128:(c + 1) * 128], scalar1=Mc, scalar2=None, op0=ALU.subtract))
            gop("dve", lambda e, c=c, Mc=Mc: e.tensor_scalar(out=hden[:, c * 128:(c + 1) * 128], in0=bb[:, c * 128:(c + 1) * 128], scalar1=Mc, scalar2=None, op0=ALU.add))
        gop("act", lambda e: e.activation(out=ww[:], in_=ww[:], func=AF.Exp))
        gop("act", lambda e: e.activation(out=hden[:], in_=hden[:], func=AF.Exp, scale=-1.0))
        gop("dve", lambda e: e.tensor_scalar(out=g1[:], in0=beta[:], scalar1=-1.0, scalar2=None, op0=ALU.mult))
        gop("dve", lambda e: e.tensor_scalar(out=g2[:], in0=eg[:], scalar1=-1.0, scalar2=None, op0=ALU.mult))
        gop("dve", lambda e: e.tensor_tensor(out=g3[:], in0=beta[:], in1=egl[:], op=ALU.mult))
        for h in range(4):
            gop("dve", lambda e, h=h: e.tensor_scalar(out=ngc[:, h, :], in0=gc[:], scalar1=ident[:4, h:h + 1], scalar2=-1.0, op0=ALU.mult, op1=ALU.mult))
        S.last_w["ngc"] = S.last_w["Gb"]
        for qi, src in enumerate((g1, g2, g3, eg, ww, hden)):
            b = S.bank()
            for c in range(NCH):
                S.op("pe", lambda e, c=c, b=b, src=src: e.transpose(ps[:, b, c * 4:(c + 1) * 4], src[:, c * 128:(c + 1) * 128], ident[:4, :4]),
                     reads=G + ["ident"], writes=["ps%d" % b] if c == 0 else [])
            S.last_w["ps%d" % b] = ("e:pe", S.cnt["e:pe"])
            evac(tok[:, qi, :], ps[:, b, :NCH * 4], ["ps%d" % b], ["tok"])
        for qi, row in enumerate((6, 5)):
            for h in range(4):
                b = S.bank()
                S.op("pe", lambda e, h=h, b=b, row=row: e.matmul(ps[:, b, :NCH], lhsT=sel4[:, h, :], rhs=sm[:, row, :], start=True, stop=True),
                     reads=G + ["sel4"], writes=["ps%d" % b])
                evac(bc[:, qi, h, :], ps[:, b, :NCH], ["ps%d" % b], ["bc"])

        S.barrier()
        stg.close()
        mstop = ""
        xin = sb("xin_m", [128, T + 3])
        cw = sb("cw", [128, 4])
        qT, kT, vT = sb("qT", [128, T]), sb("kT", [128, T]), sb("vT", [128, T])
        gT_ = sb("gateT", [128, T])
        ktok, vtok = sb("ktok", [128, NCH, 128]), sb("vtok", [128, NCH, 132])
        oT16 = sb("oT16", [128, T], BF16)
        nrm = sb("nrmw", [128, 1])
        sc = sb("sc", [128, 8])
        t1, t2, t3, t4 = sb("t1", [128, 132]), sb("t2", [128, 132]), sb("t3", [128, 132]), sb("t4", [128, 132])
        epsb = sb("epsb_m", [128, 1])
        S.op("dve", lambda e: e.memset(epsb[:], EPS), writes=["epsb_m"])
        S.op("dve", lambda e: e.memset(xin[:, 0:3], 0.0), writes=["xin_m"])

        def load_conv_silu(dst, dkey, row0, cwap, l2=False, scale=None):
            S.dma("sp", xin[:, 3:T + 3], P.zT[row0:row0 + 128, :], writes=["xin_m"], sem="xin_m")
            S.dma("sp", cw[:], cwap, writes=["cw"], sem="cw")
            S.op("dve", lambda e: e.tensor_scalar(out=dst[:], in0=xin[:, 0:T], scalar1=cw[:, 0:1], scalar2=None, op0=ALU.mult),
                 reads=["xin_m", "cw"], writes=[dkey])
            for j in range(1, 4):
                S.op("dve", lambda e, j=j: e.scalar_tensor_tensor(out=dst[:], in0=xin[:, j:T + j], scalar=cw[:, j:j + 1], in1=dst[:], op0=ALU.mult, op1=ALU.add),
                     reads=["xin_m", "cw", dkey], writes=[dkey])
            S.op("act", lambda e: e.activation(out=dst[:], in_=dst[:], func=AF.Silu), reads=[dkey], writes=[dkey])
            if l2:
                for c0 in range(0, T, 512):
                    w_ = min(512, T - c0)
                    S.op("act", lambda e, c0=c0, w_=w_: e.activation(out=xin[:, 3 + c0:3 + c0 + w_], in_=dst[:, c0:c0 + w_], func=AF.Square),
                         reads=[dkey], writes=["xin_m"])
                    b = S.bank()
                    S.op("pe", lambda e, c0=c0, w_=w_, b=b: e.matmul(ps[:, b, :w_], lhsT=P.ones32[:], rhs=xin[:, 3 + c0:3 + c0 + w_], start=True, stop=True),
                         reads=["xin_m", "ones32"], writes=["ps%d" % b])
                    S.op("act", lambda e, c0=c0, w_=w_, b=b: e.activation(out=xin[:, 3 + c0:3 + c0 + w_], in_=ps[:, b, :w_], func=AF.Sqrt, bias=epsb[:], scale=1.0),
                         reads=["ps%d" % b, "epsb_m"], writes=["xin_m"])
                    S.op("dve", lambda e, c0=c0, w_=w_: e.reciprocal(out=xin[:, 3 + c0:3 + c0 + w_], in_=xin[:, 3 + c0:3 + c0 + w_]), reads=["xin_m"], writes=["xin_m"])
                    if scale is None:
                        S.op("dve", lambda e, c0=c0, w_=w_: e.tensor_tensor(out=dst[:, c0:c0 + w_], in0=dst[:, c0:c0 + w_], in1=xin[:, 3 + c0:3 + c0 + w_], op=ALU.mult),
                             reads=["xin_m", dkey], writes=[dkey])
                    else:
                        S.op("dve", lambda e, c0=c0, w_=w_: e.scalar_tensor_tensor(out=dst[:, c0:c0 + w_], in0=dst[:, c0:c0 + w_], scalar=scale, in1=xin[:, 3 + c0:3 + c0 + w_],
                                                                                 op0=ALU.mult, op1=ALU.mult),
                             reads=["xin_m", dkey], writes=[dkey])

        def to_tok(src, skey, dst, dkey, width=128):
            for c in range(NCH):
                b = S.bank()
                P.transpose_to(src[:, c * 128:(c + 1) * 128], skey, 128, 128, b)
                evac(dst[:, c, 0:128], ps[:, b, :128], ["ps%d" % b], [dkey])

        def finish_head(otok, okey, c, wkey_scale, gate_ap, row0, extra=1.0):
            S.op("act", lambda e: e.activation(out=t4[:, 0:128], in_=otok, func=AF.Square), reads=[okey], writes=["t4"])
            S.op("dve", lambda e: e.tensor_reduce(out=sc[:, 0:1], in_=t4[:, 0:128], axis=AX.X, op=ALU.add), reads=["t4"], writes=["sc"])
            S.op("act", lambda e: e.activation(out=sc[:, 0:1], in_=sc[:, 0:1], func=AF.Sqrt, bias=epsb[:], scale=1.0 / 128), reads=["sc", "epsb_m"], writes=["sc"])
            S.op("dve", lambda e: e.reciprocal(out=sc[:, 0:1], in_=sc[:, 0:1]), reads=["sc"], writes=["sc"])
            S.op("dve", lambda e: e.tensor_scalar(out=t4[:, 0:128], in0=otok, scalar1=sc[:, 0:1], scalar2=extra, op0=ALU.mult, op1=ALU.mult),
                 reads=[okey, "sc"], writes=["t4"])
            b = S.bank()
            P.transpose_to(t4[:, 0:128], "t4", 128, 128, b)
            if gate_ap is not None:
                S.op("dve", lambda e: e.scalar_tensor_tensor(out=oT16[:, c * 128:(c + 1) * 128], in0=ps[:, b, :128], scalar=nrm[:, 0:1], in1=gate_ap,
                                                             op0=ALU.mult, op1=ALU.mult), reads=["ps%d" % b, "nrmw", "gateT"], writes=["oT16"])
            else:
                S.op("dve", lambda e: e.tensor_scalar(out=oT16[:, c * 128:(c + 1) * 128], in0=ps[:, b, :128], scalar1=nrm[:, 0:1], scalar2=None, op0=ALU.mult),
                     reads=["ps%d" % b, "nrmw"], writes=["oT16"])

        with ExitStack() as st2:
            sb2 = lambda name, shape, dt=F32: P.sb(st2, name, shape, dt)
            attnT = sb2("attnT", [128, NCH, 128])
            RT = sb2("RT", [128, NCH, 128])
            E_, Ei, Es = sb2("E_", [128, 128]), sb2("Ei", [128, 128]), sb2("Es", [128, 128])
            Pm, Qm, Rm = sb2("Pm", [128, 8, 128]), sb2("Qm", [128, 8, 128]), sb2("Rm", [128, 8, 128])
            St = sb2("St", [128, 128])
            S.dma("sp", nrm[:], I["dn_norm"][l], writes=["nrmw"], sem="nrmw")
            for h in range(NH_):
                load_conv_silu(qT, "qT", h * 128, I["conv_dn"][l, h * 128:(h + 1) * 128, :], l2=True, scale=128.0 ** -0.5)
                load_conv_silu(kT, "kT", 512 + h * 128, I["conv_dn"][l, 512 + h * 128:512 + (h + 1) * 128, :], l2=True)
                load_conv_silu(vT, "vT", 1024 + h * 128, I["conv_dn"][l, 1024 + h * 128:1024 + (h + 1) * 128, :])
                S.dma("sp", gT_[:], P.zT[2560 + h * 128:2560 + (h + 1) * 128, :], writes=["gateT"], sem="gateT")
                S.op("act", lambda e: e.activation(out=gT_[:], in_=gT_[:], func=AF.Silu), reads=["gateT"], writes=["gateT"])
                to_tok(kT, "kT", ktok, "ktok")
                to_tok(vT, "vT", vtok, "vtok")
                if mstop == "dnload":
                    continue
                GI = 4
                for cg in range(0, NCH, GI):
                    grp = list(range(cg, min(cg + GI, NCH)))
                    for ci, c in enumerate(grp):
                        cs = slice(c * 128, (c + 1) * 128)
                        col = c * 4 + h
                        q0, p0, r0 = ci * 2, ci * 2, ci * 2
                        bD = S.bank()
                        def mmD(e, bD=bD, cs=cs, h=h):
                            e.matmul(ps[:, bD, :128], lhsT=sel4[:, h, :], rhs=gc[:, cs], start=True, stop=False)
                            return e.matmul(ps[:, bD, :128], lhsT=ngc[:, h, cs], rhs=ones4[:], start=False, stop=True)
                        S.op("pe", mmD, reads=G + ["sel4", "ngc"], writes=["ps%d" % bD])
                        S.op("dve", lambda e, bD=bD: e.tensor_scalar(out=E_[:], in0=ps[:, bD, :128], scalar1=0.0, scalar2=None, op0=ALU.min), reads=["ps%d" % bD], writes=["E_"])
                        S.op("act", lambda e: e.activation(out=E_[:], in_=E_[:], func=AF.Exp), reads=["E_"], writes=["E_"])
                        S.op("pool", lambda e: e.tensor_tensor(out=Ei[:], in0=E_[:], in1=P.mui[:], op=ALU.mult), reads=["E_", "mui"], writes=["Ei"])
                        S.op("pool", lambda e: e.tensor_tensor(out=Es[:], in0=E_[:], in1=P.mus[:], op=ALU.mult), reads=["E_", "mus"], writes=["Es"])
                        bK = S.bank()
                        S.op("pe", lambda e, bK=bK, cs=cs: e.matmul(ps[:, bK, :128], lhsT=kT[:, cs], rhs=qT[:, cs], start=True, stop=True), reads=["kT", "qT"], writes=["ps%d" % bK])
                        S.op("dve", lambda e, bK=bK, c=c: e.tensor_tensor(out=attnT[:, c, :], in0=ps[:, bK, :128], in1=Ei[:], op=ALU.mult), reads=["ps%d" % bK, "Ei"], writes=["attnT"])
                        bK2 = S.bank()
                        S.op("pe", lambda e, bK2=bK2, cs=cs: e.matmul(ps[:, bK2, :128], lhsT=kT[:, cs], rhs=kT[:, cs], start=True, stop=True), reads=["kT"], writes=["ps%d" % bK2])
                        S.op("dve", lambda e, bK2=bK2, col=col, q0=q0: e.scalar_tensor_tensor(out=Qm[:, q0, :], in0=ps[:, bK2, :128], scalar=tok[:, 0, col:col + 1], in1=Es[:],
                                                                                      op0=ALU.mult, op1=ALU.mult), reads=["ps%d" % bK2, "tok", "Es"], writes=["Q%d" % q0])
                        bT = S.bank()
                        P.transpose_to(Qm[:, q0, :], "Q%d" % q0, 128, 128, bT)
                        evac(Pm[:, p0, :], ps[:, bT, :128], ["ps%d" % bT], ["P%d" % p0])
                        S.op("dve", lambda e, q0=q0, r0=r0: e.tensor_tensor(out=Rm[:, r0, :], in0=Qm[:, q0, :], in1=ident[:], op=ALU.add), reads=["Q%d" % q0, "ident"], writes=["R%d" % r0])
                    cur = 0
                    for m in range(1, 8):
                        nx = 1 - cur
                        for ci, c in enumerate(grp):
                            ic, inx = ci * 2 + cur, ci * 2 + nx
                            b1 = S.bank()
                            S.op("pe", lambda e, b1=b1, ic=ic: e.matmul(ps[:, b1, :128], lhsT=Qm[:, ic, :], rhs=Pm[:, ic, :], start=True, stop=True),
                                 reads=["Q%d" % ic, "P%d" % ic], writes=["ps%d" % b1])
                            evac(Pm[:, inx, :], ps[:, b1, :128], ["ps%d" % b1], ["P%d" % inx])
                            if m <= 6:
                                b2 = S.bank()
                                S.op("pe", lambda e, b2=b2, ic=ic: e.matmul(ps[:, b2, :128], lhsT=Pm[:, ic, :], rhs=Qm[:, ic, :], start=True, stop=True),
                                     reads=["Q%d" % ic, "P%d" % ic], writes=["ps%d" % b2])
                                evac(Qm[:, inx, :], ps[:, b2, :128], ["ps%d" % b2], ["Q%d" % inx])
                        for ci, c in enumerate(grp):
                            ic, inx = ci * 2 + cur, ci * 2 + nx
                            b3 = S.bank()
                            S.op("pe", lambda e, b3=b3, ic=ic, inx=inx: e.matmul(ps[:, b3, :128], lhsT=Pm[:, inx, :], rhs=Rm[:, ic, :], start=True, stop=True),
                                 reads=["P%d" % inx, "R%d" % ic], writes=["ps%d" % b3])
                            dst = RT[:, c, :] if m == 7 else Rm[:, inx, :]
                            S.op("dve", lambda e, b3=b3, ic=ic, dst=dst: e.tensor_tensor(out=dst, in0=ps[:, b3, :128], in1=Rm[:, ic, :], op=ALU.add),
                                 reads=["ps%d" % b3, "R%d" % ic], writes=["RT"] if m == 7 else ["R%d" % inx])
                        cur = nx
                if mstop == "dnpre":
                    continue
                S.op("dve", lambda e: e.memset(St[:], 0.0), writes=["St"])
                for c in range(NCH):
                    cs = slice(c * 128, (c + 1) * 128)
                    col = c * 4 + h
                    b1 = S.bank()
                    S.op("pe", lambda e, b1=b1, cs=cs: e.matmul(ps[:, b1, :128], lhsT=kT[:, cs], rhs=St[:], start=True, stop=True), reads=["kT", "St"], writes=["ps%d" % b1])
                    S.op("dve", lambda e, b1=b1, c=c, col=col: e.scalar_tensor_tensor(out=t1[:, 0:128], in0=ps[:, b1, :128], scalar=tok[:, 1, col:col + 1], in1=vtok[:, c, 0:128],
                                                                                     op0=ALU.mult, op1=ALU.add), reads=["ps%d" % b1, "tok", "vtok"], writes=["t1"])
                    b2 = S.bank()
                    S.op("pe", lambda e, b2=b2, c=c: e.matmul(ps[:, b2, :128], lhsT=RT[:, c, :], rhs=t1[:, 0:128], start=True, stop=True), reads=["RT", "t1"], writes=["ps%d" % b2])
                    S.op("dve", lambda e, b2=b2, col=col: e.tensor_scalar(out=t2[:, 0:128], in0=ps[:, b2, :128], scalar1=tok[:, 0, col:col + 1], scalar2=-1.0, op0=ALU.mult, op1=ALU.mult),
                         reads=["ps%d" % b2, "tok"], writes=["t2"])
                    S.op("dve", lambda e, b2=b2, col=col: e.tensor_scalar(out=t3[:, 0:128], in0=ps[:, b2, :128], scalar1=tok[:, 2, col:col + 1], scalar2=None, op0=ALU.mult),
                         reads=["ps%d" % b2, "tok"], writes=["t3"])
                    b3 = S.bank()
                    S.op("pe", lambda e, b3=b3, cs=cs: e.matmul(ps[:, b3, :128], lhsT=qT[:, cs], rhs=St[:], start=True, stop=True), reads=["qT", "St"], writes=["ps%d" % b3])
                    b4 = S.bank()
                    S.op("pe", lambda e, b4=b4, c=c: e.matmul(ps[:, b4, :128], lhsT=attnT[:, c, :], rhs=t2[:, 0:128], start=True, stop=True), reads=["attnT", "t2"], writes=["ps%d" % b4])
                    b5 = S.bank()
                    S.op("pe", lambda e, b5=b5, c=c: e.matmul(ps[:, b5, :128], lhsT=ktok[:, c, :], rhs=t3[:, 0:128], start=True, stop=True), reads=["ktok", "t3"], writes=["ps%d" % b5])
                    S.op("dve", lambda e, b5=b5, c=c, h=h: e.scalar_tensor_tensor(out=St[:], in0=St[:], scalar=bc[:, 0, h, c:c + 1], in1=ps[:, b5, :128], op0=ALU.mult, op1=ALU.add),
                         reads=["St", "bc", "ps%d" % b5], writes=["St"])
                    evac(t1[:, 0:128], ps[:, b4, :128], ["ps%d" % b4], ["t1"], eng="act")
                    S.op("dve", lambda e, b3=b3, col=col: e.scalar_tensor_tensor(out=t2[:, 0:128], in0=ps[:, b3, :128], scalar=tok[:, 3, col:col + 1], in1=t1[:, 0:128],
                                                                                 op0=ALU.mult, op1=ALU.add), reads=["ps%d" % b3, "tok", "t1"], writes=["t2"])
                    finish_head(t2[:, 0:128], "t2", c, None, gT_[:, cs], 0)
                S.dma("sp", P.mixT[h * 128:(h + 1) * 128, :], oT16[:], reads=["oT16"], sem="oT16")

        if mstop in ("dn", "dnload", "dnpre"):
            S.barrier()
            return
        with ExitStack() as st2:
            sb2 = lambda name, shape, dt=F32: P.sb(st2, name, shape, dt)
            stp = sb2("stp", [128, 132])
            sT = sb2("sT", [128, 128])
            muis = sb2("muis", [128, 128])
            S.op("dve", lambda e: e.tensor_scalar(out=muis[:], in0=P.mui[:], scalar1=128.0 ** -0.5, scalar2=None, op0=ALU.mult), reads=["mui"], writes=["muis"])
            S.dma("sp", nrm[:], I["ml_norm"][l], reads=[], writes=["nrmw"], sem="nrmw")
            for h in range(NH_):
                load_conv_silu(qT, "qT", 1536 + h * 128, I["conv_ml"][l, h * 128:(h + 1) * 128, :])
                load_conv_silu(kT, "kT", 2048 + h * 128, I["conv_ml"][l, 512 + h * 128:512 + (h + 1) * 128, :])
                S.dma("sp", vT[:], P.zT[6144 + h * 128:6144 + (h + 1) * 128, :], writes=["vT"], sem="vT")
                S.dma("sp", gT_[:], P.zT[6656 + h * 128:6656 + (h + 1) * 128, :], writes=["gateT"], sem="gateT")
                S.op("act", lambda e: e.activation(out=gT_[:], in_=gT_[:], func=AF.Sigmoid), reads=["gateT"], writes=["gateT"])
                to_tok(kT, "kT", ktok, "ktok")
                to_tok(vT, "vT", vtok, "vtok")
                S.op("dve", lambda e: e.memset(vtok[:, :, 128:129], 1.0), reads=[], writes=["vtok"])
                S.op("dve", lambda e: e.memset(stp[:], 0.0), writes=["stp"])
                for c in range(NCH):
                    cs = slice(c * 128, (c + 1) * 128)
                    col = c * 4 + h
                    b1 = S.bank()
                    S.op("pe", lambda e, b1=b1, cs=cs: e.matmul(ps[:, b1, :128], lhsT=kT[:, cs], rhs=qT[:, cs], start=True, stop=True), reads=["kT", "qT"], writes=["ps%d" % b1])
                    S.op("dve", lambda e, b1=b1, col=col: e.scalar_tensor_tensor(out=sT[:], in0=ps[:, b1, :128], scalar=tok[:, 4, col:col + 1], in1=muis[:], op0=ALU.mult, op1=ALU.mult),
                         reads=["ps%d" % b1, "tok", "muis"], writes=["sT"])
                    b2 = S.bank()
                    def mm2(e, b2=b2, c=c, cs=cs):
                        e.matmul(ps[:, b2, :129], lhsT=sT[:], rhs=vtok[:, c, 0:129], start=True, stop=False)
                        return e.matmul(ps[:, b2, :129], lhsT=qT[:, cs], rhs=stp[:, 0:129], start=False, stop=True)
                    S.op("pe", mm2, reads=["sT", "vtok", "qT", "stp"], writes=["ps%d" % b2])
                    S.op("dve", lambda e, c=c, col=col: e.tensor_scalar(out=t1[:, 0:129], in0=vtok[:, c, 0:129], scalar1=tok[:, 4, col:col + 1], scalar2=128.0 ** -0.5, op0=ALU.mult, op1=ALU.mult),
                         reads=["vtok", "tok"], writes=["t1"])
                    b3 = S.bank()
                    S.op("pe", lambda e, b3=b3, c=c: e.matmul(ps[:, b3, :129], lhsT=ktok[:, c, :], rhs=t1[:, 0:129], start=True, stop=True), reads=["ktok", "t1"], writes=["ps%d" % b3])
                    if c + 1 < NCH:
                        S.op("dve", lambda e, b3=b3: e.tensor_tensor(out=stp[:, 0:129], in0=ps[:, b3, :129], in1=stp[:, 0:129], op=ALU.add), reads=["ps%d" % b3, "stp"], writes=["stp"])
                        S.op("dve", lambda e, c=c, h=h: e.tensor_scalar(out=stp[:, 0:129], in0=stp[:, 0:129], scalar1=bc[:, 1, h, c + 1:c + 2], scalar2=None, op0=ALU.mult),
                             reads=["stp", "bc"], writes=["stp"])
                    S.op("act", lambda e, b2=b2, col=col: e.activation(out=sc[:, 1:2], in_=ps[:, b2, 128:129], func=AF.Abs),
                         reads=["ps%d" % b2, "tok"], writes=["sc1"])
                    S.op("dve", lambda e, col=col: e.tensor_tensor(out=sc[:, 1:2], in0=sc[:, 1:2], in1=tok[:, 5, col:col + 1], op=ALU.max),
                         reads=["sc1", "tok"], writes=["sc1"])
                    S.op("dve", lambda e: e.reciprocal(out=sc[:, 1:2], in_=sc[:, 1:2]), reads=["sc1"], writes=["sc1"])
                    S.op("dve", lambda e, b2=b2: e.tensor_scalar(out=t2[:, 0:128], in0=ps[:, b2, 0:128], scalar1=sc[:, 1:2], scalar2=None, op0=ALU.mult), reads=["ps%d" % b2, "sc1"], writes=["t2"])
                    finish_head(t2[:, 0:128], "t2", c, None, gT_[:, cs], 0)
                S.dma("sp", P.mixT[1536 + h * 128:1536 + (h + 1) * 128, :], oT16[:], reads=["oT16"], sem="oT16")

        if mstop == "ml":
            S.barrier()
            return
        with ExitStack() as st2:
            sb2 = lambda name, shape, dt=F32: P.sb(st2, name, shape, dt)
            QG = cfg.QG
            NG = T // QG
            NS = QG // 128
            qa = sb2("qa", [67, 2, T], BF16)
            ka = sb2("ka", [67, 2, T], BF16)
            v1 = sb2("v1", [128, NCH, 132], BF16)
            PT = sb2("PT", [128, NCH, QG], BF16)
            om = sb2("om", [128, 2, NS, 128])
            lam = sb2("lam", [128, 8])
            lq = sb2("lq", [128, 4, 64])
            for i, nm in enumerate(("diff_lq1", "diff_lk1", "diff_lq2", "diff_lk2")):
                S.dma("sp", lq[:, i, :], I[nm][l:l + 1, :].broadcast_to([128, 64]),
                      writes=["lq"], sem="lq")
            S.last_w["lq"] = (S.dsem("lq"), S.cnt[S.dsem("lq")])
            S.op("dve", lambda e: e.tensor_tensor(out=lq[:, 0, :], in0=lq[:, 0, :], in1=lq[:, 1, :], op=ALU.mult), reads=["lq"], writes=["lq"])
            S.op("dve", lambda e: e.tensor_tensor(out=lq[:, 2, :], in0=lq[:, 2, :], in1=lq[:, 3, :], op=ALU.mult), reads=["lq"], writes=["lq"])
            S.op("dve", lambda e: e.tensor_reduce(out=lam[:, 0:1], in_=lq[:, 0, :], axis=AX.X, op=ALU.add), reads=["lq"], writes=["lam"])
            S.op("dve", lambda e: e.tensor_reduce(out=lam[:, 1:2], in_=lq[:, 2, :], axis=AX.X, op=ALU.add), reads=["lq"], writes=["lam"])
            S.op("act", lambda e: e.activation(out=lam[:, 0:2], in_=lam[:, 0:2], func=AF.Exp), reads=["lam"], writes=["lam"])
            S.op("dve", lambda e: e.tensor_tensor(out=lam[:, 2:3], in0=lam[:, 1:2], in1=lam[:, 0:1], op=ALU.subtract), reads=["lam"], writes=["lam"])
            S.op("dve", lambda e: e.tensor_scalar(out=lam[:, 2:3], in0=lam[:, 2:3], scalar1=-lambda_init, scalar2=None, op0=ALU.add), reads=["lam"], writes=["lam"])
            S.dma("sp", nrm[:], I["diff_norm"][l], writes=["nrmw"], sem="nrmw")
            for h in range(8):
                slope = 2.0 ** (-8.0 * (h + 1) / 8)
                for m in range(2):
                    r = h * 128 + m * 64
                    S.dma("sp", xin[0:64, 3:T + 3], P.zT[3072 + r:3072 + r + 64, :], writes=["xin_m"], sem="xin_m")
                    S.op("act", lambda e, m=m: e.activation(out=qa[0:64, m, :], in_=xin[0:64, 3:T + 3], func=AF.Copy, scale=0.125), reads=["xin_m"], writes=["qa"])
                    S.dma("sp", xin[0:64, 3:T + 3], P.zT[4096 + r:4096 + r + 64, :], writes=["xin_m"], sem="xin_m")
                    S.op("act", lambda e, m=m: e.activation(out=ka[0:64, m, :], in_=xin[0:64, 3:T + 3], func=AF.Copy), reads=["xin_m"], writes=["ka"])
                    S.dma("pool", qa[64:67, m, :], I["c_qaug"][h], writes=["qa"], sem="qa")
                    S.dma("pool", ka[64:67, m, :], I["c_kaug"][h], writes=["ka"], sem="ka")
                S.dma("sp", vT[:], P.zT[5120 + h * 128:5120 + (h + 1) * 128, :], writes=["vT"], sem="vT")
                for c in range(NCH):
                    b = S.bank()
                    P.transpose_to(vT[:, c * 128:(c + 1) * 128], "vT", 128, 128, b)
                    evac(v1[:, c, 0:128], ps[:, b, :128], ["ps%d" % b], ["v1"])
                S.op("dve", lambda e: e.memset(v1[:, :, 128:129], 1.0), writes=["v1"])
                for g in range(NG):
                    nJ = (g + 1) * NS
                    for m in range(2):
                        for J in range(nJ):
                            r = J - g * NS
                            col0 = max(r, 0) * 128
                            ncol = QG - col0
                            cGJ = -slope * (g * QG - J * 128)
                            b = S.bank()
                            S.op("pe", lambda e, b=b, J=J, m=m, g=g, col0=col0, ncol=ncol: e.matmul(ps[:, b, :ncol], lhsT=ka[:, m, J * 128:(J + 1) * 128],
                                                                                                 rhs=qa[:, m, g * QG + col0:(g + 1) * QG], start=True, stop=True),
                                 reads=["qa", "ka"], writes=["ps%d" % b])
                            S.op("act", lambda e, b=b, J=J, col0=col0, ncol=ncol, cGJ=cGJ: e.activation(out=PT[:, J, col0:QG], in_=ps[:, b, :ncol], func=AF.Exp, bias=float(cGJ), scale=1.0),
                                 reads=["ps%d" % b], writes=["PT%d" % J])
                            if r >= 0:
                                S.op("pool", lambda e, J=J, col0=col0: e.tensor_tensor(out=PT[:, J, col0:col0 + 128], in0=PT[:, J, col0:col0 + 128], in1=P.mui16[:], op=ALU.mult),
                                     reads=["PT%d" % J, "mui16"], writes=["PT%d" % J])
                        for i in range(NS):
                            nK = g * NS + i + 1
                            b = S.bank()
                            def mmpv(e, b=b, i=i, nK=nK):
                                ins = None
                                for J in range(nK):
                                    ins = e.matmul(ps[:, b, :129], lhsT=PT[:, J, i * 128:(i + 1) * 128], rhs=v1[:, J, 0:129], start=(J == 0), stop=(J == nK - 1))
                                return ins
                            S.op("pe", mmpv, reads=["PT%d" % J for J in range(nK)] + ["v1"], writes=["ps%d" % b])
                            S.op("dve", lambda e, b=b: e.reciprocal(out=sc[:, 2:3], in_=ps[:, b, 128:129]), reads=["ps%d" % b], writes=["sc2"])
                            S.op("dve", lambda e, b=b, m=m, i=i: e.tensor_scalar(out=om[:, m, i, :], in0=ps[:, b, 0:128], scalar1=sc[:, 2:3], scalar2=None, op0=ALU.mult),
                                 reads=["ps%d" % b, "sc2"], writes=["om%d_%d" % (m, i)])
                    for i in range(NS):
                        S.op("dve", lambda e, i=i: e.scalar_tensor_tensor(out=t2[:, 0:128], in0=om[:, 1, i, :], scalar=lam[:, 2:3], in1=om[:, 0, i, :], op0=ALU.mult, op1=ALU.add),
                             reads=["om0_%d" % i, "om1_%d" % i, "lam"], writes=["t2"])
                        finish_head(t2[:, 0:128], "t2", g * NS + i, None, None, 0, extra=(1.0 - lambda_init))
                S.dma("sp", P.mixT[512 + h * 128:512 + (h + 1) * 128, :], oT16[:], reads=["oT16"], sem="oT16")
        S.barrier()


NH_ = 4
_bias_cache = {}


def P_bias(P, S, val):
    return P.cbias_ap(val)


def _consts(T):
    j = np.arange(128)[:, None]
    i = np.arange(128)[None, :]
    c = {}
    c["c_ident"] = np.eye(128, dtype=np.float32)
    c["c_mui"] = (i >= j).astype(np.float32)
    c["c_mus"] = (i > j).astype(np.float32)
    cm = np.ones((4, T), np.float32)
    cm[:, ::128] = 0
    c["c_cmask"] = cm
    QG = min(512, T)
    pos = np.arange(T)
    qa = np.zeros((8, 3, T), np.float32)
    ka = np.zeros((8, 3, T), np.float32)
    for h in range(8):
        s = 2.0 ** (-8.0 * (h + 1) / 8)
        a = pos % QG
        qa[h, 0] = -s * (a % 128)
        qa[h, 1] = -s * 128 * (a // 128)
        qa[h, 2] = 1.0
        ka[h, 0] = 1.0
        ka[h, 1] = 1.0
        ka[h, 2] = s * (pos % 128)
    c["c_qaug"] = qa
    c["c_kaug"] = ka
    return c


def _make_maps(inp, cfg, ncores):
    L = cfg.L
    cst = _consts(cfg.T)
    sh = {}
    sh["conv_dn"] = np.ascontiguousarray(np.transpose(inp["conv_dn"], (0, 2, 1)))
    sh["conv_ml"] = np.ascontiguousarray(np.transpose(inp["conv_ml"], (0, 2, 1)))
    for nm in ("dn_a_log", "dn_dt_bias", "ml_i_bias", "ml_f_bias", "dn_norm", "diff_norm", "ml_norm"):
        sh[nm] = np.ascontiguousarray(inp[nm][..., None])
    for nm in ("attn_norm", "ffn_norm", "ple_norm"):
        sh[nm] = np.ascontiguousarray(inp[nm].reshape(L, -1, 128).transpose(0, 2, 1))
    sh["final_norm"] = np.ascontiguousarray(inp["final_norm"].reshape(-1, 128).T)
    base = {}
    for k, v in inp.items():
        if k in ("x", "p"):
            continue
        base[k] = sh[k] if k in sh else np.ascontiguousarray(v)
    base.update(cst)
    maps = []
    for b in range(ncores):
        m = dict(base)
        m["x"] = np.ascontiguousarray(inp["x"][b])
        m["p"] = np.ascontiguousarray(inp["p"][:, b])
        maps.append(m)
    return maps


def kernel(**inputs):
    inp = {k: np.asarray(v, dtype=np.float32) for k, v in inputs.items()}
    B, T, D = inp["x"].shape
    L = inp["w_in"].shape[0]
    FF = inp["dense_w_gate"].shape[2]
    cfg = Cfg(D=D, T=T, FF=FF, L=L, TQ=min(512, T))
    prog = Prog(cfg)
    nc = prog.build()
    maps = _make_maps(inp, cfg, B)
    res = run_bass_kernel_spmd(nc, maps, core_ids=list(range(B)))
    return np.stack([np.asarray(r["out"], dtype=np.float32) for r in res.results])
```
